# Optimizing a Trainium2 kernel written in Bass

```python
import jax, jax.numpy as jnp
from jax import lax
import numpy as np

D_MODEL = 4096
BATCH = 2
SEQ = 8192
DEPTH = 2

MLA_HEADS = 16
MLA_Q_RANK = 768
MLA_KV_RANK = 512
MLA_NOPE = 128
MLA_ROPE = 64
MLA_V = 128
ROPE_THETA = 10000.0
GLA_HEADS = 4
GLA_DK = 128
GLA_DV = 256
GLA_GATE_RANK = 16
GLA_GATE_NORM = 16.0
GLA_CHUNK = 64
DSA_HEADS = 8
DSA_KV_HEADS = 2
DSA_DH = 128
IDX_HEADS = 32
IDX_DH = 64
IDX_TOPK = 256
Q_BLOCK = 128
NORM_EPS = 1e-6
NEG = -1e30

MLA_WIDTH = MLA_HEADS * MLA_V
GLA_WIDTH = GLA_HEADS * GLA_DV
DSA_WIDTH = DSA_HEADS * DSA_DH
N_BRANCH = 3

IN_SPLITS = (
    MLA_Q_RANK, MLA_KV_RANK, MLA_ROPE,
    GLA_HEADS * GLA_DK, GLA_HEADS * GLA_DK, GLA_WIDTH, GLA_GATE_RANK,
    DSA_WIDTH, DSA_KV_HEADS * DSA_DH, DSA_KV_HEADS * DSA_DH,
    IDX_HEADS * IDX_DH, IDX_DH, IDX_HEADS,
    MLA_WIDTH, GLA_WIDTH, DSA_WIDTH,
)
IN_WIDTH = sum(IN_SPLITS)

kernel_name = "hybrid_mla_gla_dsa_gated_merge"


def rms_norm(x, g):
    xf = x.astype(jnp.float32)
    y = xf * lax.rsqrt(jnp.mean(xf * xf, axis=-1, keepdims=True) + NORM_EPS)
    return (y * g.astype(jnp.float32)).astype(x.dtype)


def rope_tables(positions, dim):
    inv = ROPE_THETA ** (-jnp.arange(0, dim, 2, dtype=jnp.float32) / dim)
    ang = positions.astype(jnp.float32)[..., None] * inv
    return jnp.cos(ang), jnp.sin(ang)


def apply_rope(x, cos, sin):
    xf = x.astype(jnp.float32)
    x1, x2 = jnp.split(xf, 2, axis=-1)
    c = cos[:, :, None, :]
    s = sin[:, :, None, :]
    return jnp.concatenate([x1 * c - x2 * s, x1 * s + x2 * c], axis=-1).astype(x.dtype)


def causal_block_attention(q, k, v, scale):
    B, S, H, _ = q.shape
    key_idx = jnp.arange(S)
    qs = q * scale

    def block(i):
        start = i * Q_BLOCK
        qb = lax.dynamic_slice_in_dim(qs, start, Q_BLOCK, axis=1)
        s = jnp.einsum('bqhd,bkhd->bhqk', qb, k, preferred_element_type=jnp.float32)
        q_idx = start + jnp.arange(Q_BLOCK)
        mask = key_idx[None, :] <= q_idx[:, None]
        p = jax.nn.softmax(jnp.where(mask[None, None], s, NEG), axis=-1)
        return jnp.einsum('bhqk,bkhd->bqhd', p.astype(v.dtype), v)

    out = lax.map(block, jnp.arange(S // Q_BLOCK))
    return out.transpose(1, 0, 2, 3, 4).reshape(B, S, H * v.shape[-1])


def mla_mixer(cq, ckv, krope, cos, sin, g_q, w_uq, g_kv, w_ukv):
    B, S, _ = cq.shape
    q = (rms_norm(cq, g_q) @ w_uq).reshape(B, S, MLA_HEADS, MLA_NOPE + MLA_ROPE)
    q = jnp.concatenate([q[..., :MLA_NOPE], apply_rope(q[..., MLA_NOPE:], cos, sin)], axis=-1)
    kv = (rms_norm(ckv, g_kv) @ w_ukv).reshape(B, S, MLA_HEADS, MLA_NOPE + MLA_V)
    k_nope, v = kv[..., :MLA_NOPE], kv[..., MLA_NOPE:]
    k_r = apply_rope(krope[:, :, None, :], cos, sin)
    k = jnp.concatenate([k_nope, jnp.broadcast_to(k_r, (B, S, MLA_HEADS, MLA_ROPE))], axis=-1)
    return causal_block_attention(q, k, v, (MLA_NOPE + MLA_ROPE) ** -0.5)


def gla_mixer(q, k, v, g_low, w_g2, b_g, g_out):
    B, S, _ = q.shape
    H, DK, DV, C = GLA_HEADS, GLA_DK, GLA_DV, GLA_CHUNK
    N = S // C
    f32 = jnp.float32

    def chunks(t, d):
        return t.astype(f32).reshape(B, N, C, H, d).transpose(1, 0, 3, 2, 4)

    qc = chunks(q, DK) * DK ** -0.5
    kc = chunks(k, DK)
    vc = chunks(v, DV)
    glog = jax.nn.log_sigmoid((g_low @ w_g2 + b_g).astype(f32)) / GLA_GATE_NORM
    bc = jnp.cumsum(chunks(glog, DK), axis=3)
    b_last = bc[:, :, :, -1:, :]
    q_dec = qc * jnp.exp(bc)
    k_inv = kc * jnp.exp(-bc)
    k_end = kc * jnp.exp(b_last - bc)
    decay = jnp.exp(b_last[:, :, :, 0, :])
    tril = jnp.arange(C)[:, None] >= jnp.arange(C)[None, :]
    attn = jnp.where(tril, jnp.einsum('nbhcd,nbhsd->nbhcs', q_dec, k_inv), 0.0)
    o_intra = jnp.einsum('nbhcs,nbhse->nbhce', attn, vc)

    def step(state, inp):
        qd, ke, vv, dec = inp
        o = jnp.einsum('bhcd,bhde->bhce', qd, state)
        state = dec[..., None] * state + jnp.einsum('bhcd,bhce->bhde', ke, vv)
        return state, o

    state0 = jnp.zeros((B, H, DK, DV), f32)
    _, o_inter = lax.scan(step, state0, (q_dec, k_end, vc, decay))
    o = (o_intra + o_inter).transpose(1, 0, 3, 2, 4).reshape(B, S, H, DV)
    o = rms_norm(o, g_out)
    return o.reshape(B, S, H * DV).astype(q.dtype)


def dsa_mixer(q, k, v, iq, ik, iw):
    B, S, _ = q.shape
    REP = DSA_HEADS // DSA_KV_HEADS
    top_k = min(IDX_TOPK, S // 4)
    q = q.reshape(B, S, DSA_KV_HEADS, REP, DSA_DH) * DSA_DH ** -0.5
    k = k.reshape(B, S, DSA_KV_HEADS, DSA_DH)
    v = v.reshape(B, S, DSA_KV_HEADS, DSA_DH)
    iq = iq.reshape(B, S, IDX_HEADS, IDX_DH) * IDX_DH ** -0.5
    iw = iw.astype(jnp.float32) * IDX_HEADS ** -0.5
    key_idx = jnp.arange(S)
    gather = jax.vmap(lambda tb, ib: jnp.take(tb, ib, axis=0))

    def block(i):
        start = i * Q_BLOCK
        q_idx = start + jnp.arange(Q_BLOCK)
        iqb = lax.dynamic_slice_in_dim(iq, start, Q_BLOCK, axis=1)
        iwb = lax.dynamic_slice_in_dim(iw, start, Q_BLOCK, axis=1)
        qb = lax.dynamic_slice_in_dim(q, start, Q_BLOCK, axis=1)
        rel = jax.nn.relu(jnp.einsum('bqhd,bkd->bqhk', iqb, ik, preferred_element_type=jnp.float32))
        score = jnp.einsum('bqhk,bqh->bqk', rel, iwb)
        causal = key_idx[None, :] <= q_idx[:, None]
        score = jnp.where(causal[None], score, NEG)
        _, idx = lax.top_k(score, top_k)
        valid = idx <= q_idx[None, :, None]
        ksel = gather(k, idx)
        vsel = gather(v, idx)
        s = jnp.einsum('bqgrd,bqkgd->bqgrk', qb, ksel, preferred_element_type=jnp.float32)
        p = jax.nn.softmax(jnp.where(valid[:, :, None, None, :], s, NEG), axis=-1)
        o = jnp.einsum('bqgrk,bqkgd->bqgrd', p.astype(vsel.dtype), vsel)
        return o.reshape(B, Q_BLOCK, DSA_WIDTH)

    out = lax.map(block, jnp.arange(S // Q_BLOCK))
    return out.transpose(1, 0, 2, 3).reshape(B, S, DSA_WIDTH)


def hybrid_layer(x, c, cos, sin, norm_g, w_ada, b_ada, w_in, mla_gq, mla_wuq, mla_gkv,
                 mla_wukv, gla_wg2, gla_bg, gla_gout, w_mg, b_mg, w_bm, w_bg, w_bd, w_o):
    mod = jax.nn.silu(c) @ w_ada + b_ada
    shift, scale, gate = jnp.split(mod, 3, axis=-1)
    h = rms_norm(x, norm_g) * (1.0 + scale[:, None, :]) + shift[:, None, :]

    proj = h @ w_in
    (cq, ckv, krope, gq, gk, gv, glow, dq, dk, dv, iq, ik, iw,
     z_mla, z_gla, z_dsa) = jnp.split(proj, list(np.cumsum(IN_SPLITS)[:-1]), axis=-1)

    y_mla = mla_mixer(cq, ckv, krope, cos, sin, mla_gq, mla_wuq, mla_gkv, mla_wukv) * jax.nn.silu(z_mla)
    y_gla = gla_mixer(gq, gk, gv, glow, gla_wg2, gla_bg, gla_gout) * jax.nn.silu(z_gla)
    y_dsa = dsa_mixer(dq, dk, dv, iq, ik, iw) * jax.nn.silu(z_dsa)

    g_a, g_b, g_c = jnp.split(jax.nn.sigmoid(h @ w_mg + b_mg), N_BRANCH, axis=-1)
    merged = g_a * (y_mla @ w_bm) + g_b * (y_gla @ w_bg) + g_c * (y_dsa @ w_bd)
    return x + gate[:, None, :] * (merged @ w_o)


def setup_inputs(seed: int = 0) -> dict:
    key = jax.random.key(seed)
    ks = jax.random.split(key, 24)
    f32 = jnp.float32
    D, L = D_MODEL, DEPTH

    def nrm(k, shape, fan_in):
        return jax.random.normal(k, shape, f32) * fan_in ** -0.5

    def gain(k, shape):
        return 1.0 + 0.1 * jax.random.normal(k, shape, f32)

    offset = jax.random.randint(ks[2], (BATCH, 1), 0, 1024, dtype=jnp.int32)
    positions = offset + jnp.arange(SEQ, dtype=jnp.int32)[None, :]
    return {
        "x": jax.random.normal(ks[0], (BATCH, SEQ, D), f32),
        "c": jax.random.normal(ks[1], (BATCH, D), f32),
        "positions": positions,
        "norm_g": gain(ks[3], (L, D)),
        "w_ada": nrm(ks[4], (L, D, 3 * D), D),
        "b_ada": 0.02 * jax.random.normal(ks[5], (L, 3 * D), f32),
        "w_in": nrm(ks[6], (L, D, IN_WIDTH), D),
        "mla_gq": gain(ks[7], (L, MLA_Q_RANK)),
        "mla_wuq": nrm(ks[8], (L, MLA_Q_RANK, MLA_HEADS * (MLA_NOPE + MLA_ROPE)), MLA_Q_RANK),
        "mla_gkv": gain(ks[9], (L, MLA_KV_RANK)),
        "mla_wukv": nrm(ks[10], (L, MLA_KV_RANK, MLA_HEADS * (MLA_NOPE + MLA_V)), MLA_KV_RANK),
        "gla_wg2": nrm(ks[11], (L, GLA_GATE_RANK, GLA_HEADS * GLA_DK), GLA_GATE_RANK),
        "gla_bg": 0.02 * jax.random.normal(ks[12], (L, GLA_HEADS * GLA_DK), f32),
        "gla_gout": gain(ks[13], (L, GLA_DV)),
        "w_mg": nrm(ks[14], (L, D, N_BRANCH * D), D),
        "b_mg": 0.02 * jax.random.normal(ks[15], (L, N_BRANCH * D), f32),
        "w_bm": nrm(ks[16], (L, MLA_WIDTH, D), MLA_WIDTH),
        "w_bg": nrm(ks[17], (L, GLA_WIDTH, D), GLA_WIDTH),
        "w_bd": nrm(ks[18], (L, DSA_WIDTH, D), DSA_WIDTH),
        "w_o": nrm(ks[19], (L, D, D), D),
        "final_g": gain(ks[20], (D,)),
    }


def reference(x, c, positions, norm_g, w_ada, b_ada, w_in, mla_gq, mla_wuq, mla_gkv, mla_wukv,
              gla_wg2, gla_bg, gla_gout, w_mg, b_mg, w_bm, w_bg, w_bd, w_o, final_g):
    cos, sin = rope_tables(positions, MLA_ROPE)
    for l in range(DEPTH):
        x = hybrid_layer(x, c, cos, sin, norm_g[l], w_ada[l], b_ada[l], w_in[l],
                         mla_gq[l], mla_wuq[l], mla_gkv[l], mla_wukv[l],
                         gla_wg2[l], gla_bg[l], gla_gout[l], w_mg[l], b_mg[l],
                         w_bm[l], w_bg[l], w_bd[l], w_o[l])
    return rms_norm(x, final_g)
```

```python
import numpy as np
from contextlib import ExitStack
import concourse.bass as bass
import concourse.mybir as mybir
from concourse.bass_utils import run_bass_kernel_spmd

F32 = mybir.dt.float32
BF16 = mybir.dt.bfloat16
I32 = mybir.dt.int32
AF = mybir.ActivationFunctionType
ALU = mybir.AluOpType
AX = mybir.AxisListType

D = 4096
SEQ = 8192
NT = 2048
DEPTH = 2
EPS = 1e-6
GROUPS4 = [[0, 1, 2, 3], [4, 5, 6, 7]]
GROUP8 = [list(range(8))]
PAIRS = [[0, 4], [1, 5], [2, 6], [3, 7]]
PI = float(np.pi)


class Sched:
    COMPUTE = ("pe", "act", "dve", "pool")

    def __init__(self, nc, es, n_dma_sems=8):
        self.nc = nc
        self.es = es
        self.epoch = 0
        self.items = {k: [] for k in ("pe", "act", "dve", "pool", "sp")}
        self.sems = {}
        self.cnt = {}
        for k in self.COMPUTE:
            self.sems[k] = es.enter_context(nc.semaphore("s_" + k))
            self.cnt[k] = 0
        self.dq = {}
        for q in ("sp", "pool", "act"):
            ss = [es.enter_context(nc.semaphore("d_%s%d" % (q, i))) for i in range(n_dma_sems)]
            for i, s in enumerate(ss):
                self.sems[("d", q, i)] = s
                self.cnt[("d", q, i)] = 0
            self.dq[q] = 0
        self.sems["cc"] = es.enter_context(nc.semaphore("s_cc"))
        self.cnt["cc"] = 0
        self.nd = n_dma_sems
        self.seen = {k: {} for k in self.items}
        self.st = {}
        self.n_wait = 0
        self.n_ins = 0

    def _need(self, reads, writes, eng=None):
        deps = []
        for k in reads:
            s = self.st.get(k)
            if s and s[0]:
                deps.append(s[0])
        for k in writes:
            s = self.st.get(k)
            if s:
                if s[0] and s[0][0] != eng:
                    deps.append(s[0])
                deps.extend(t for t in s[1] if t[0] != eng)
        return deps

    def _wait(self, eng, deps):
        seen = self.seen[eng]
        best = {}
        for (sk, c) in deps:
            if eng == "pe" and sk == "pe":
                continue
            if seen.get(sk, 0) >= c:
                continue
            if best.get(sk, 0) < c:
                best[sk] = c
        for sk, c in best.items():
            self.items[eng].append(("w", self.sems[sk], c))
            seen[sk] = c
            self.n_wait += 1

    def _record(self, ticket, reads, writes):
        for k in reads:
            s = self.st.setdefault(k, [None, []])
            s[1].append(ticket)
            if len(s[1]) > 64:
                m = {}
                for (sk, c) in s[1]:
                    if m.get(sk, 0) < c:
                        m[sk] = c
                s[1] = list(m.items())
        for k in writes:
            self.st[k] = [ticket, []]

    def op(self, eng, fn, reads=(), writes=()):
        psr = [k for k in reads if isinstance(k, tuple) and k[0] == "ps"]
        if psr:
            writes = list(writes) + psr
        self._wait(eng, self._need(reads, writes, eng))
        self.cnt[eng] += 1
        t = (eng, self.cnt[eng])
        self.items[eng].append(("i", fn, self.sems[eng], 1))
        self._record(t, reads, writes)
        self.n_ins += 1
        return t

    def dma(self, q, out, in_, reads=(), writes=(), **kw):
        n = self.dq[q]
        self.dq[q] += 1
        sk = ("d", q, n % self.nd)
        deps = self._need(reads, writes)
        if self.cnt[sk] > 0:
            deps.append((sk, self.cnt[sk]))
        self._wait(q, deps)
        self.cnt[sk] += 16
        t = (sk, self.cnt[sk])
        self.items[q].append(("i", lambda e: e.dma_start(out=(out(e) if callable(out) else out),
                                                         in_=(in_(e) if callable(in_) else in_), **kw), self.sems[sk], 16))
        self._record(t, reads, writes)
        self.n_ins += 1
        return t

    def collective(self, kind, groups, in_ap, out_ap, reads=(), writes=()):
        self._wait("pool", self._need(reads, writes))
        self.cnt["cc"] += 1
        t = ("cc", self.cnt["cc"])
        self.items["pool"].append(("c", lambda e: e.collective_compute(
            kind, ALU.bypass, replica_groups=groups, ins=[in_ap], outs=[out_ap]), self.sems["cc"], None))
        self._record(t, reads, writes)
        return t

    def barrier(self):
        allt = [(sk, c) for sk, c in self.cnt.items() if c > 0]
        for eng in self.items:
            self._wait(eng, allt)
        self.st = {}
        self.epoch += 1
        for k in self.COMPUTE:
            if self.cnt[k] > 0:
                self.sems[k] = self.es.enter_context(self.nc.semaphore("s_%s_%d" % (k, self.epoch)))
                self.cnt[k] = 0
                for eng in self.seen:
                    self.seen[eng].pop(k, None)

    def wait_all(self, eng):
        self._wait(eng, [(sk, c) for sk, c in self.cnt.items() if c > 0])

    def emit(self, block):
        def run(e, items):
            for it in items:
                if it[0] == "w":
                    e.wait_ge(it[1], it[2])
                elif it[0] == "i":
                    it[1](e).then_inc(it[2], it[3])
                else:
                    it[1](e).then_inc(it[2])

        @block.tensor
        def _(e):
            run(e, self.items["pe"])

        @block.scalar
        def _(e):
            run(e, self.items["act"])

        @block.vector
        def _(e):
            run(e, self.items["dve"])

        @block.gpsimd
        def _(e):
            run(e, self.items["pool"])

        @block.sync
        def _(e):
            run(e, self.items["sp"])


class Arena:
    def __init__(self, tensor_f32, nwords):
        self.t = tensor_f32
        self.n = nwords
        self.off = 0
        self.marks = []

    def alloc(self, nelem, dtype=F32, parts=128):
        words = (nelem + 1) // 2 if dtype == BF16 else nelem
        a = self.off
        self.off += words
        assert self.off <= self.n, "SBUF arena overflow %d > %d" % (self.off, self.n)
        v = self.t[0:parts, a:a + words]
        if dtype != F32:
            v = v.bitcast(dtype)
        return v

    def mark(self):
        self.marks.append(self.off)

    def release(self):
        self.off = self.marks.pop()


class Rot:
    def __init__(self, n):
        self.n = n
        self.i = 0

    def next(self):
        v = self.i % self.n
        self.i += 1
        return v


C_CQ, C_CKV, C_KR, C_GQ, C_GK, C_GV, C_GLOW = 0, 768, 1280, 1344, 1856, 2368, 3392
C_DQ, C_DK, C_DV, C_IQ, C_IK, C_IW, C_Z = 3408, 4432, 4688, 4944, 6992, 7056, 7088
IN_W = 11184

WIN_UNITS = [
    ("cq0", [(0, 512)]), ("cq1", [(512, 768)]), ("ckv", [(768, 1280)]),
    ("kr", [(1280, 1344), (1312, 1344), (1280, 1312), (3392, 3408)]),
    ("gq", [(1344, 1856)]), ("gk", [(1856, 2368)]),
    ("gv0", [(2368, 2880)]), ("gv1", [(2880, 3392)]),
    ("dq0", [(3408, 3920)]), ("dq1", [(3920, 4432)]),
    ("dkik", [(4432, 4688), (6992, 7056)]),
    ("dviw", [(4688, 4944), (7056, 7088)]),
    ("iq0", [(4944, 5456)]), ("iq1", [(5456, 5968)]), ("iq2", [(5968, 6480)]), ("iq3", [(6480, 6992)]),
] + [("z%d" % i, [(C_Z + 512 * i, C_Z + 512 * (i + 1))]) for i in range(8)]


def units_plain(n_cols):
    return [("u%d" % i, [(512 * i, min(n_cols, 512 * (i + 1)))]) for i in range((n_cols + 511) // 512)]


def unit_width(u):
    return sum(b - a for a, b in u[1])


TILED = {
    "w_ada": (4096, 12288, units_plain(12288)),
    "w_in": (4096, IN_W, WIN_UNITS),
    "w_mg": (4096, 12288, units_plain(12288)),
    "w_bm": (2048, 4096, units_plain(4096)),
    "w_bg": (1024, 4096, units_plain(4096)),
    "w_bd": (1024, 4096, units_plain(4096)),
    "w_o": (4096, 4096, units_plain(4096)),
}
PLAIN = {"mla_wuq": (768, 3072), "mla_wukv": (512, 4096)}

COL_INV, COL_SGN, COL_NEGPI = 0, 1, 2
COL_L0 = 4
COL_NORMG, COL_BSHIFT, COL_BSCALE, COL_BMG, COL_GQ, COL_GKV = 0, 32, 64, 96, 192, 198
COL_PER_L = 208
NCOL = COL_L0 + DEPTH * COL_PER_L


def col_of(l, what, i=0):
    return COL_L0 + l * COL_PER_L + what + i


class Builder:
    def __init__(self, layers=(0, 1), taps=(), stop_after=None):
        self.layers = tuple(layers)
        self.taps = set(taps)
        self.stop_after = stop_after
        self.nc = bass.Bass("TRN2", target_bir_lowering=False)
        self.ins = {}
        self.outs = {}
        self.dr = {}

    def inp(self, name, shape, dt=F32):
        t = self.nc.dram_tensor(name, list(shape), dt, kind="ExternalInput")
        self.ins[name] = (tuple(shape), dt)
        return t

    def outp(self, name, shape, dt=F32):
        t = self.nc.dram_tensor(name, list(shape), dt, kind="ExternalOutput")
        self.outs[name] = (tuple(shape), dt)
        return t

    def dram(self, name, shape, dt):
        t = self.nc.dram_tensor(name, list(shape), dt)
        self.dr[name] = t
        return t

    def rank_of(self, e):
        if not hasattr(self, "_rk"):
            self._rk = {}
        k = id(e)
        if k not in self._rk:
            self._rk[k] = e.partition_id() % 4
        return self._rk[k]

    def build(self):
        nc = self.nc
        with ExitStack() as es:
            self.es = es
            AW = 51200
            self.arena_t = es.enter_context(nc.sbuf_tensor("arena", [128, AW], F32))
            self.ar = Arena(self.arena_t, AW)
            self.ps = [es.enter_context(nc.psum_tensor("ps%d" % i, [128, 512], F32)) for i in range(8)]
            self.S = Sched(nc, es)
            self.declare_io()
            self.consts()
            self.phase_W()
            self.phase_R()
            for l in self.layers:
                if self.stop_after == ("W",):
                    break
                self.phase_M(l)
                if self.stop_after == ("M", l):
                    break
                self.phase_A(l)
                if self.stop_after == ("A", l):
                    break
                self.phase_G1(l)
                import os
                if not os.environ.get("SKIP_MLA"):
                    self.phase_MLA(l)
                if self.stop_after == ("MLA", l):
                    break
                self.phase_DSA(l)
                if self.stop_after == ("DSA", l):
                    break
                self.phase_GLA(l)
                if self.stop_after == ("GLA", l):
                    break
                self.phase_G2(l)
                self.phase_C(l, last=(l == DEPTH - 1))
                if self.stop_after == ("C", l):
                    break
            self.S.barrier()
            if self.taps:
                od = self.outp("tap_done", [128, 4])
                self.S.dma("sp", od[:, :], self.cols[:, 0:4], reads=["cols"])
            self.S.wait_all("sp")
            self.S.wait_all("pool")
            with nc.Block() as block:
                self.S.emit(block)
        return nc

    def declare_io(self):
        self.x_own = self.inp("x_own", [NT, D])
        self.c_own = self.inp("c_own", [128, 32])
        self.pos_perm = self.inp("pos_perm", [1, SEQ], I32)
        self.cols_in = self.inp("cols", [128, NCOL])
        self.ident_in = self.inp("ident", [128, 128])
        self.mla_mask_in = self.inp("mla_mask", [128, 2048], BF16)
        self.dsa_dmask_in = self.inp("dsa_dmask", [128, 512])
        self.dsa_c01_in = self.inp("dsa_c01", [128, 512], BF16)
        self.gla_consts_in = self.inp("gla_consts", [64, 192])
        self.final_g_in = self.inp("final_g_row", [1, D])
        self.gla_wg2_in = {}
        self.gla_gout_in = {}
        for l in self.layers:
            self.gla_wg2_in[l] = self.inp("gla_wg2_%d" % l, [17, 128])
            self.gla_gout_in[l] = self.inp("gla_gout_%d" % l, [1, 256])
        self.out_own = self.outp("out_own", [NT, D])
        self.xmid = self.dram("xmid", [NT, D], F32)
        self.rows_in = {}
        for l in self.layers:
            self.rows_in[("bgate", l)] = self.inp("bgate_%d" % l, [1, D])
        self.wsh = {}
        for l in self.layers:
            for name, (K, N, units) in TILED.items():
                self.wsh[(name, l)] = self.inp("%s_%d" % (name, l), [K // 8, N])
            self.wsh[("wuq_own", l)] = self.inp("wuq_own_%d" % l, [768, 1024])
            self.wsh[("wukv_own", l)] = self.inp("wukv_own_%d" % l, [512, 1024])

    def consts(self):
        S, ar = self.S, self.ar
        self.cols = ar.alloc(NCOL)
        S.dma("sp", self.cols, self.cols_in[:, :], writes=["cols"])
        self.ident = ar.alloc(128)
        S.dma("sp", self.ident, self.ident_in[:, :], writes=["ident"])
        self.ones_f = ar.alloc(128)
        S.op("dve", lambda e: e.memset(self.ones_f, 1.0), writes=["ones_f"])
        self.ident_b = ar.alloc(128, BF16)
        S.op("dve", lambda e: e.tensor_copy(out=self.ident_b, in_=self.ident), reads=["ident"], writes=["ident_b"])
        self.ones_b = ar.alloc(128, BF16)
        S.op("dve", lambda e: e.memset(self.ones_b, 1.0), writes=["ones_b"])
        self.a_col = ar.alloc(32)
        self.b_col = ar.alloc(32)

    def gather2(self, name, rows, cols, dt, src_key):
        S = self.S
        wb = self.dr[name + "_b"]
        wm = self.dram(name + "_m", [2 * rows, cols], dt)
        wt = self.dram(name + "_t", [8 * rows, cols], dt)
        S.collective("AllGather", PAIRS, wb.ap().opt(), wm.ap().opt(), reads=[src_key], writes=[("wm", name)])
        S.collective("AllGather", GROUPS4, wm.ap().opt(), wt.ap().opt(), reads=[("wm", name)], writes=[("wt", name)])
        return wt, ("wt", name)

    def phase_W(self):
        S, ar, nc = self.S, self.ar, self.nc
        ar.mark()
        CW = 2048
        nslot = 2
        stg_f = [ar.alloc(4 * CW) for _ in range(nslot)]
        stg_b = [ar.alloc(4 * CW, BF16) for _ in range(nslot)]
        rot = Rot(nslot)
        cast_rot = Rot(2)
        self.WTG = {}
        for l in self.layers:
            for name, (K, N, units) in TILED.items():
                KL = K // 1024
                NU = len(units)
                per_group = 16 // KL
                upp = 8 // KL
                src = self.wsh[(name, l)]
                srcv = src.ap().rearrange("(k p) n -> p k n", p=128)
                groups = []
                for g0 in range(0, NU, per_group):
                    ng = min(per_group, NU - g0)
                    gname = "w_%s_%d_%d" % (name, l, g0)
                    wb = self.dram(gname + "_b", [ng * 128, KL * 512], BF16)
                    groups.append([gname, g0, ng, wb, {}])
                self.WTG[(name, l)] = (groups, per_group, KL, upp)
                pending = {}
                ui = 0
                while ui < NU:
                    grp = []
                    w = 0
                    while ui < NU and w + unit_width(units[ui]) <= CW:
                        grp.append(ui)
                        w += unit_width(units[ui])
                        ui += 1
                    s = rot.next()
                    kf, kb = ("wstg_f", s), ("wstg_b", s)
                    sf = stg_f[s][:, 0:KL * w].rearrange("p (k c) -> p k c", k=KL)
                    sb = stg_b[s][:, 0:KL * w].rearrange("p (k c) -> p k c", k=KL)
                    off = 0
                    offs = {}
                    for u in grp:
                        offs[u] = off
                        for (a, b) in units[u][1]:
                            S.dma("sp", sf[:, :, off:off + (b - a)], srcv[:, :, a:b], writes=[kf])
                            off += b - a
                    ce = ("dve", "act")[cast_rot.next()]
                    if ce == "dve":
                        S.op("dve", lambda e, sb=sb, sf=sf: e.tensor_copy(out=sb, in_=sf), reads=[kf], writes=[kb])
                    else:
                        S.op("act", lambda e, sb=sb, sf=sf: e.activation(out=sb, in_=sf, func=AF.Copy), reads=[kf], writes=[kb])
                    for u in grp:
                        wu = unit_width(units[u])
                        G = groups[u // per_group]
                        j = u % per_group
                        wbv = G[3].ap().rearrange("(j p) f -> j p f", p=128)
                        S.dma("pool", wbv[j, :, 0:KL * wu].rearrange("p (k c) -> p k c", k=KL),
                              sb[:, :, offs[u]:offs[u] + wu], reads=[kb], writes=[("wbu", G[0], j)])
                        pending[G[0]] = pending.get(G[0], 0) + 1
                        if pending[G[0]] == G[2]:
                            ng = G[2]
                            S_keys = [("wbu", G[0], jj) for jj in range(ng)]
                            wm = self.dram(G[0] + "_m", [2 * ng * 128, KL * 512], BF16)
                            S.collective("AllGather", PAIRS, G[3].ap().opt(), wm.ap().opt(), reads=S_keys,
                                         writes=[("wm", G[0])])
                            for q in range(2):
                                for pi_, p0 in enumerate(range(0, ng, upp)):
                                    np_ = min(upp, ng - p0)
                                    wt = self.dram("%s_t%d_%d" % (G[0], q, pi_), [4 * np_ * 128, KL * 512], BF16)
                                    r0 = (q * ng + p0) * 128
                                    S.collective("AllGather", GROUPS4, wm[r0:r0 + np_ * 128, :], wt.ap().opt(),
                                                 reads=[("wm", G[0])], writes=[("wt", name, l, G[0], q, pi_)])
                                    G[4][(q, pi_)] = (wt, np_)
        S.barrier()
        ar.release()

    def load_unit(self, q_, dst, name, l, u, width, writes):
        groups, per_group, KL, upp = self.WTG[(name, l)]
        G = groups[u // per_group]
        j = u % per_group
        pi_, jj = j // upp, j % upp
        d5 = dst.rearrange("p (r q k) c -> p r q (k c)", r=4, q=2)
        t = None
        for q in range(2):
            wt, np_ = G[4][(q, pi_)]
            v = wt.ap().rearrange("(r j p) f -> j p r f", r=4, j=np_, p=128)
            t = self.S.dma(q_, d5[:, :, q, :], v[jj, :, :, 0:KL * width], reads=[("wt", name, l)], writes=writes)
        return t

    def phase_M(self, l):
        S, ar, ps = self.S, self.ar, self.ps
        ar.mark()
        gate_bc = ar.alloc(D)
        sc = ar.alloc(32)
        scb = ar.alloc(32, BF16)
        screp = ar.alloc(32 * 128, BF16)
        ws = [ar.alloc(32 * 512, BF16) for _ in range(2)]
        modc = ar.alloc(64)
        bg = ar.alloc(D)
        import os
        MCUT = int(os.environ.get("MCUT", "99"))
        if MCUT == -10:
            S.barrier(); ar.release(); return
        S.dma("sp", sc, self.c_own[:, :], writes=["m_sc"])
        if MCUT == -11:
            S.barrier(); ar.release(); return
        S.dma("sp", bg, self.rows_in[("bgate", l)][0:1, :].partition_broadcast(128), writes=["m_bg"])
        if MCUT == -12:
            S.barrier(); ar.release(); return
        S.op("act", lambda e: e.activation(out=sc, in_=sc, func=AF.Silu), reads=["m_sc"], writes=["m_sc"])
        S.op("dve", lambda e: e.tensor_copy(out=scb, in_=sc), reads=["m_sc"], writes=["m_scb"])
        if MCUT == -13:
            S.barrier(); ar.release(); return
        screp3 = screp.rearrange("p (k m) -> p k m", k=32)
        for kc in range(32):
            S.op("dve", lambda e, kc=kc: e.tensor_copy(out=screp3[:, kc, :], in_=scb[:, kc:kc + 1].to_broadcast([128, 128])),
                 reads=["m_scb"], writes=["m_screp"])
        rot = Rot(2)
        for u in range(24):
            if MCUT < 1:
                break
            s = rot.next()
            w3 = ws[s].rearrange("p (k c) -> p k c", k=32)
            self.load_unit("sp", w3, "w_ada", l, u, 512, writes=[("m_ws", s)])
            if MCUT < 2:
                continue
            if u < 16:
                for pc in range(4):
                    j = u * 4 + pc
                    bank = 4 + (j % 2)
                    for kc in range(32):
                        S.op("pe", lambda e, kc=kc, pc=pc, w3=w3, bank=bank: e.matmul(
                            ps[bank][:, 0:1], lhsT=w3[:, kc, pc * 128:(pc + 1) * 128], rhs=scb[:, kc:kc + 1],
                            start=(kc == 0), stop=(kc == 31)),
                            reads=[("m_ws", s), "m_scb"], writes=[("ps", bank)])
                    S.op("dve", lambda e, j=j, bank=bank: e.tensor_copy(out=modc[:, j:j + 1], in_=ps[bank][:, 0:1]),
                         reads=[("ps", bank)], writes=["m_modc"])
            else:
                j = u - 16
                bank = 6 + (j % 2)
                for kc in range(32):
                    S.op("pe", lambda e, kc=kc, w3=w3, bank=bank: e.matmul(
                        ps[bank][:, :], lhsT=screp3[:, kc, :], rhs=w3[:, kc, :],
                        start=(kc == 0), stop=(kc == 31)),
                        reads=[("m_ws", s), "m_screp"], writes=[("ps", bank)])
                S.op("dve", lambda e, j=j, bank=bank: e.tensor_tensor(
                    out=gate_bc[:, j * 512:(j + 1) * 512], in0=ps[bank][:, :], in1=bg[:, j * 512:(j + 1) * 512],
                    op=ALU.add), reads=[("ps", bank), "m_bg"], writes=["gate_bc"])
        if MCUT == -1:
            S.barrier()
            ar.release()
            return
        cN = self.cols[:, col_of(l, COL_NORMG):col_of(l, COL_NORMG) + 32]
        cBsh = self.cols[:, col_of(l, COL_BSHIFT):col_of(l, COL_BSHIFT) + 32]
        cBsc = self.cols[:, col_of(l, COL_BSCALE):col_of(l, COL_BSCALE) + 32]
        S.op("dve", lambda e: e.tensor_tensor(out=self.b_col, in0=modc[:, 0:32], in1=cBsh, op=ALU.add),
             reads=["m_modc", "cols"], writes=["b_col"])
        S.op("dve", lambda e: e.scalar_tensor_tensor(out=self.a_col, in0=modc[:, 32:64], scalar=1.0, in1=cBsc,
                                                     op0=ALU.add, op1=ALU.add),
             reads=["m_modc", "cols"], writes=["a_col"])
        S.op("dve", lambda e: e.tensor_tensor(out=self.a_col, in0=self.a_col, in1=cN, op=ALU.mult),
             reads=["a_col", "cols"], writes=["a_col"])
        if MCUT == -2:
            S.barrier()
            ar.release()
            return
        gdr = self.dram("gatebc_%d" % l, [128, D], F32)
        S.dma("sp", gdr[:, :], gate_bc, reads=["gate_bc"], writes=["gatebc_dram"])
        if MCUT == -3:
            S.barrier()
            ar.release()
            return
        if ("mod", l) in self.taps:
            if MCUT == -6:
                o = self.outp("tap_b_%d" % l, [128, 32])
                S.dma("sp", o[:, :], self.b_col, reads=["b_col"])
            elif MCUT == -7:
                o = self.outp("tap_b_%d" % l, [128, 32])
                bn = self.dram("bounce_tap", [128, 32], F32)
                S.dma("sp", bn[:, :], self.cols[:, 0:32], reads=["cols"], writes=["bnc"])
                S.dma("sp", o[:, :], bn[:, :], reads=["bnc"])
            elif MCUT != -5:
                o = self.outp("tap_mod_%d" % l, [128, 64])
                S.dma("sp", o[:, 0:32], self.a_col, reads=["a_col"])
                S.dma("sp", o[:, 32:64], self.b_col, reads=["b_col"])
            if MCUT not in (-4, -6, -7):
                o2 = self.outp("tap_gate_%d" % l, [128, D])
                S.dma("sp", o2[:, :], gate_bc, reads=["gate_bc"])
        S.barrier()
        ar.release()

    def phase_A(self, l):
        S, ar, ps, nc = self.S, self.ar, self.ps, self.nc
        ar.mark()
        GA_ROWS = 768 + 512 + 512 + 512 + 256 + 64
        self.GA_ROWS = GA_ROWS
        self.GA_OFF = dict(cq=0, ckv=768, gq=1280, gk=1792, dk=2304, ik=2560)
        self.GAF_OFF = dict(kr1=0, kr2=64, glow=128)
        self.GAT_OFF = dict(gk=0, gv=512, dv=1536)
        ga = self.dram("ga_src_%d" % l, [GA_ROWS, NT], BF16)
        gaf = self.dram("gaf_src_%d" % l, [144, NT], F32)
        gat = self.dram("gat_src_%d" % l, [NT, 1792], BF16)
        dqT = self.dram("dqT_%d" % l, [1024, NT], BF16)
        iqT = self.dram("iqT_%d" % l, [2048, NT], BF16)
        zT = self.dram("zT_%d" % l, [4096, NT], BF16)
        gT = self.dram("gT_%d" % l, [12288, NT], BF16)
        iw = self.dram("iw_%d" % l, [NT, 32], F32)
        self.A_out = dict(ga=ga, gaf=gaf, gat=gat, dqT=dqT, iqT=iqT, zT=zT, gT=gT, iw=iw)
        A_meta = dict(ga=([GA_ROWS, NT], BF16), gaf=([144, NT], F32), gat=([NT, 1792], BF16), dqT=([1024, NT], BF16),
                      iqT=([2048, NT], BF16), zT=([4096, NT], BF16), gT=([12288, NT], BF16), iw=([NT, 32], F32))
        xsrc = self.x_own if l == 0 else self.dr["xres%d" % (l - 1)]

        hT = ar.alloc(32 * 1024, BF16)
        hT3 = hT.rearrange("p (k t) -> p k t", k=32)
        ws = [ar.alloc(32 * 512, BF16) for _ in range(2)]
        X = ar.alloc(D)
        junk = ar.alloc(D, BF16)
        ss = ar.alloc(4)
        stg = [ar.alloc(512, BF16) for _ in range(4)]
        stgf = [ar.alloc(512) for _ in range(2)]
        raw = ar.alloc(6 * 512)
        raw3 = raw.rearrange("p (a c) -> p a c", a=6)
        sq = [ar.alloc(512) for _ in range(2)]
        rstd_bc = ar.alloc(512)
        wrot, srot, sfrot, brot, sqrot, evrot = Rot(2), Rot(4), Rot(2), Rot(6), Rot(2), Rot(2)

        def evac_copy(dst, src, reads, writes, force=None):
            if force == "act" or (force is None and evrot.next() == 0):
                S.op("act", lambda e: e.activation(out=dst, in_=src, func=AF.Copy), reads=reads, writes=writes)
            else:
                S.op("dve", lambda e: e.tensor_copy(out=dst, in_=src), reads=reads, writes=writes)

        unit_idx = {u[0]: i for i, u in enumerate(WIN_UNITS)}

        for half in range(2):
            for tt in range(8):
                u0 = half * 1024 + tt * 128
                for hh in range(2):
                    S.dma("sp", X[:, hh * 2048:(hh + 1) * 2048], xsrc[u0:u0 + 128, hh * 2048:(hh + 1) * 2048], writes=["xt"])
                S.op("act", lambda e: e.activation(out=junk, in_=X, func=AF.Square, accum_out=ss[:, 0:1]),
                     reads=["xt"], writes=["junk", "ss0"])
                S.op("act", lambda e: e.activation(out=ss[:, 1:2], in_=ss[:, 0:1], func=AF.Sqrt, scale=1.0 / D, bias=EPS),
                     reads=["ss0"], writes=["ss1"])
                S.op("dve", lambda e: e.reciprocal(out=ss[:, 2:3], in_=ss[:, 1:2]), reads=["ss1"], writes=["ss2"])
                S.op("dve", lambda e: e.tensor_scalar(out=X, in0=X, scalar1=ss[:, 2:3], scalar2=None, op0=ALU.mult),
                     reads=["ss2", "xt"], writes=["xt"])
                for k4 in range(8):
                    bank = k4 % 2 + 6
                    for q in range(4):
                        kc = k4 * 4 + q
                        S.op("pe", lambda e, kc=kc, q=q, bank=bank: e.transpose(
                            out=ps[bank][:, q * 128:(q + 1) * 128], in_=X[:, kc * 128:(kc + 1) * 128],
                            identity=self.ident), reads=["xt", "ident"], writes=[("ps", bank)])
                    for q in range(4):
                        kc = k4 * 4 + q
                        S.op("act", lambda e, kc=kc, q=q, bank=bank, tt=tt: e.activation(
                            out=hT3[:, kc, tt * 128:(tt + 1) * 128], in_=ps[bank][:, q * 128:(q + 1) * 128],
                            func=AF.Identity, scale=self.a_col[:, kc:kc + 1], bias=self.b_col[:, kc:kc + 1]),
                            reads=[("ps", bank), "a_col", "b_col"], writes=[("hT", tt)])
            hreads = [("hT", tt) for tt in range(8)]
            if ("hT", l) in self.taps and half == 0:
                o = self.outp("tap_hT_%d" % l, [128, 32 * 1024], BF16)
                for kc in range(32):
                    S.dma("sp", o[:, kc * 1024:(kc + 1) * 1024], hT3[:, kc, :], reads=hreads)

            def fm_group(w3, wkey, c0, m, tc):
                bank = brot.next()
                for kc in range(32):
                    S.op("pe", lambda e, kc=kc: e.matmul(ps[bank][0:m, :], lhsT=w3[:, kc, c0:c0 + m],
                                                         rhs=hT3[:, kc, tc * 512:(tc + 1) * 512],
                                                         start=(kc == 0), stop=(kc == 31)),
                         reads=[wkey] + hreads, writes=[("ps", bank)])
                return bank

            def tok0(tc):
                return half * 1024 + tc * 512

            def load_win(uname):
                ui = unit_idx[uname]
                width = unit_width(WIN_UNITS[ui])
                s = wrot.next()
                w3 = ws[s][:, 0:32 * width].rearrange("p (k c) -> p k c", k=32)
                self.load_unit("sp", w3, "w_in", l, ui, width, writes=[("ws", s)])
                return w3, ("ws", s)

            def stats_group(pieces, n_feat, gcol, row0):
                for tc in range(2):
                    for pi, (w3, wkey, c0) in enumerate(pieces):
                        bank = fm_group(w3, wkey, c0, 128, tc)
                        evac_copy(raw3[:, pi, :], ps[bank][:, :], [("ps", bank)], [("raw", pi)])
                    bank = brot.next()
                    for pi in range(len(pieces)):
                        q = sqrot.next()
                        S.op("act", lambda e, pi=pi, q=q: e.activation(out=sq[q], in_=raw3[:, pi, :], func=AF.Square),
                             reads=[("raw", pi)], writes=[("sq", q)])
                        S.op("pe", lambda e, q=q, pi=pi, n=len(pieces), bank=bank: e.matmul(
                            ps[bank][:, :], lhsT=self.ones_f, rhs=sq[q], start=(pi == 0), stop=(pi == n - 1)),
                            reads=[("sq", q), "ones_f"], writes=[("ps", bank)])
                    S.op("act", lambda e, bank=bank: e.activation(
                        out=rstd_bc, in_=ps[bank][:, :], func=AF.Sqrt, scale=1.0 / n_feat, bias=EPS),
                        reads=[("ps", bank)], writes=["rstd_bc"])
                    S.op("dve", lambda e: e.reciprocal(out=rstd_bc, in_=rstd_bc), reads=["rstd_bc"], writes=["rstd_bc"])
                    for pi in range(len(pieces)):
                        so = srot.next()
                        S.op("dve", lambda e, pi=pi, so=so: e.scalar_tensor_tensor(
                            out=stg[so], in0=raw3[:, pi, :], scalar=self.cols[:, gcol + pi:gcol + pi + 1],
                            in1=rstd_bc, op0=ALU.mult, op1=ALU.mult),
                            reads=[("raw", pi), "rstd_bc", "cols"], writes=[("stg", so)])
                        S.dma("pool", ga[row0 + pi * 128:row0 + (pi + 1) * 128, tok0(tc):tok0(tc) + 512], stg[so],
                              reads=[("stg", so)], writes=["ga"])

            def fm_pieces(w3, wkey, pieces, func=None):
                for tc in range(2):
                    for (c0, m, dst, dkey, row0) in pieces:
                        bank = fm_group(w3, wkey, c0, m, tc)
                        so = srot.next()
                        if func is not None:
                            S.op("act", lambda e, so=so, bank=bank, m=m: e.activation(
                                out=stg[so][0:m, :], in_=ps[bank][0:m, :], func=func),
                                reads=[("ps", bank)], writes=[("stg", so)])
                        else:
                            evac_copy(stg[so][0:m, :], ps[bank][0:m, :], [("ps", bank)], [("stg", so)])
                        S.dma("pool", dst[row0:row0 + m, tok0(tc):tok0(tc) + 512], stg[so][0:m, :],
                              reads=[("stg", so)], writes=[dkey])

            def tm_unit(w3, wkey, dests, n):
                for tt in range(8):
                    u0 = half * 1024 + tt * 128
                    bank = brot.next()
                    for kc in range(32):
                        S.op("pe", lambda e, kc=kc, bank=bank, tt=tt: e.matmul(
                            ps[bank][:, 0:n], lhsT=hT3[:, kc, tt * 128:(tt + 1) * 128], rhs=w3[:, kc, 0:n],
                            start=(kc == 0), stop=(kc == 31)),
                            reads=[wkey] + hreads, writes=[("ps", bank)])
                    frc = "act" if len(dests) > 1 else None
                    for (c0, w, dst, dkey, dcol, dt) in dests:
                        if dt == BF16:
                            so = srot.next()
                            evac_copy(stg[so][:, 0:w], ps[bank][:, c0:c0 + w], [("ps", bank)], [("stg", so)], force=frc)
                            S.dma("pool", dst[u0:u0 + 128, dcol:dcol + w], stg[so][:, 0:w], reads=[("stg", so)],
                                  writes=[dkey])
                        else:
                            so = sfrot.next()
                            evac_copy(stgf[so][:, 0:w], ps[bank][:, c0:c0 + w], [("ps", bank)], [("stgf", so)], force=frc)
                            S.dma("pool", dst[u0:u0 + 128, dcol:dcol + w], stgf[so][:, 0:w], reads=[("stgf", so)],
                                  writes=[dkey])

            import os
            ACUT = float(os.environ.get("ACUT", "99"))
            if ACUT <= 1:
                break
            wa, ka = load_win("cq0")
            wb_, kb_ = load_win("cq1")
            stats_group([(wa, ka, 0), (wa, ka, 128), (wa, ka, 256), (wa, ka, 384), (wb_, kb_, 0), (wb_, kb_, 128)],
                        768.0, col_of(l, COL_GQ), self.GA_OFF["cq"])
            if ACUT <= 2:
                break
            wa, ka = load_win("ckv")
            stats_group([(wa, ka, 128 * i) for i in range(4)], 512.0, col_of(l, COL_GKV), self.GA_OFF["ckv"])
            wa, ka = load_win("kr")
            for tc in range(2):
                for (c0, m, row0) in ((0, 64, 0), (64, 64, 64), (128, 16, 128)):
                    bank = fm_group(wa, ka, c0, m, tc)
                    so = sfrot.next()
                    evac_copy(stgf[so][0:m, :], ps[bank][0:m, :], [("ps", bank)], [("stgf", so)])
                    S.dma("pool", gaf[row0:row0 + m, tok0(tc):tok0(tc) + 512], stgf[so][0:m, :],
                          reads=[("stgf", so)], writes=["gaf"])
            if ACUT <= 3:
                break
            wa, ka = load_win("gq")
            fm_pieces(wa, ka, [(i * 128, 128, ga, "ga", self.GA_OFF["gq"] + i * 128) for i in range(4)])
            wa, ka = load_win("gk")
            fm_pieces(wa, ka, [(i * 128, 128, ga, "ga", self.GA_OFF["gk"] + i * 128) for i in range(4)])
            tm_unit(wa, ka, [(0, 512, gat, "gat", self.GAT_OFF["gk"], BF16)], 512)
            if ACUT <= 4:
                break
            ASKIP = os.environ.get("ASKIP", "").split(",")
            for j in range(2):
                if "gv" in ASKIP:
                    continue
                wa, ka = load_win("gv%d" % j)
                tm_unit(wa, ka, [(0, 512, gat, "gat", self.GAT_OFF["gv"] + 512 * j, BF16)], 512)
            if ACUT <= 4.5:
                break
            for j in range(2):
                if "dq%d" % j in ASKIP:
                    continue
                wa, ka = load_win("dq%d" % j)
                fm_pieces(wa, ka, [(i * 128, 128, dqT, "dqT", 512 * j + i * 128) for i in range(4)])
            if ACUT <= 5:
                break
            wa, ka = load_win("dkik")
            fm_pieces(wa, ka, [(0, 128, ga, "ga", self.GA_OFF["dk"]), (128, 128, ga, "ga", self.GA_OFF["dk"] + 128),
                               (256, 64, ga, "ga", self.GA_OFF["ik"])])
            wa, ka = load_win("dviw")
            tm_unit(wa, ka, [(0, 256, gat, "gat", self.GAT_OFF["dv"], BF16), (256, 32, iw, "iw", 0, F32)], 288)
            if ACUT <= 6:
                break
            for j in range(4):
                wa, ka = load_win("iq%d" % j)
                fm_pieces(wa, ka, [(i * 128, 128, iqT, "iqT", 512 * j + i * 128) for i in range(4)])
            for j in range(8):
                wa, ka = load_win("z%d" % j)
                fm_pieces(wa, ka, [(i * 128, 128, zT, "zT", 512 * j + i * 128) for i in range(4)], func=AF.Silu)
            if ACUT <= 7:
                break
            for ui in range(24):
                s = wrot.next()
                w3 = ws[s].rearrange("p (k c) -> p k c", k=32)
                wkey = ("ws", s)
                self.load_unit("sp", w3, "w_mg", l, ui, 512, writes=[wkey])
                for tc in range(2):
                    for pc in range(4):
                        j = ui * 4 + pc
                        bank = fm_group(w3, wkey, pc * 128, 128, tc)
                        so = srot.next()
                        bcol = col_of(l, COL_BMG, j)
                        S.op("act", lambda e, so=so, bank=bank, bcol=bcol: e.activation(
                            out=stg[so], in_=ps[bank][:, :], func=AF.Sigmoid, bias=self.cols[:, bcol:bcol + 1]),
                            reads=[("ps", bank), "cols"], writes=[("stg", so)])
                        S.dma("pool", gT[j * 128:(j + 1) * 128, tok0(tc):tok0(tc) + 512], stg[so],
                              reads=[("stg", so)], writes=["gT"])
        for nm in ("ga", "gaf", "gat", "dqT", "iqT", "zT", "gT", "iw"):
            for tp in self.taps:
                if tp[0] == nm and tp[1] == l:
                    t = self.A_out[nm]
                    shape, dt = A_meta[nm]
                    rows = tp[2] if len(tp) > 2 else shape[0]
                    o = self.outp("tap_%s_%d" % (nm, l), [rows, shape[1]], dt)
                    step = 128
                    for r0 in range(0, rows, step):
                        r1 = min(rows, r0 + step)
                        S.dma("sp", o[r0:r1, :], t[r0:r1, :], reads=[nm])
        S.barrier()
        ar.release()


    def phase_R(self):
        S, ar = self.S, self.ar
        ar.mark()
        self.ropeC = self.dram("ropeC", [64, SEQ], F32)
        self.ropeS = self.dram("ropeS", [64, SEQ], F32)
        CH = 2048
        pi_ = ar.alloc(CH)
        pf = ar.alloc(CH)
        t1 = ar.alloc(CH)
        t2 = ar.alloc(CH)
        kf = ar.alloc(CH)
        ki_ = ar.alloc(CH)
        for c in range(SEQ // CH):
            pi32 = pi_.bitcast(I32)
            S.dma("sp", pi32[0:64, :], self.pos_perm[0:1, c * CH:(c + 1) * CH].partition_broadcast(64), writes=["r_pi"])
            S.op("dve", lambda e, pi32=pi32: e.tensor_copy(out=pf[0:64, :], in_=pi32[0:64, :]), reads=["r_pi"], writes=["r_pf"])
            S.op("dve", lambda e: e.tensor_scalar(out=pf[0:64, :], in0=pf[0:64, :], scalar1=self.cols[0:64, COL_INV:COL_INV + 1],
                                                  scalar2=None, op0=ALU.mult), reads=["r_pf", "cols"], writes=["r_pf"])
            ki = ki_.bitcast(I32)
            for (tt_, phi, dst, sgn) in ((t1, 0.5 * PI, self.ropeC, False), (t2, 0.0, self.ropeS, True)):
                key = "r_t1" if tt_ is t1 else "r_t2"
                S.op("dve", lambda e, tt_=tt_, phi=phi: e.tensor_scalar(
                    out=tt_[0:64, :], in0=pf[0:64, :], scalar1=1.0 / (2 * PI), scalar2=phi / (2 * PI) + 0.5,
                    op0=ALU.mult, op1=ALU.add), reads=["r_pf"], writes=[key])
                S.op("dve", lambda e, tt_=tt_: e.tensor_copy(out=ki[0:64, :], in_=tt_[0:64, :]), reads=[key], writes=["r_ki"])
                S.op("dve", lambda e: e.tensor_copy(out=kf[0:64, :], in_=ki[0:64, :]), reads=["r_ki"], writes=["r_kf"])
                S.op("dve", lambda e, tt_=tt_: e.scalar_tensor_tensor(
                    out=tt_[0:64, :], in0=tt_[0:64, :], scalar=-0.5, in1=kf[0:64, :], op0=ALU.add, op1=ALU.subtract),
                    reads=[key, "r_kf"], writes=[key])
                S.op("dve", lambda e, tt_=tt_: e.scalar_tensor_tensor(
                    out=tt_[0:64, :], in0=tt_[0:64, :], scalar=-0.5, in1=tt_[0:64, :], op0=ALU.is_lt, op1=ALU.add),
                    reads=[key], writes=[key])
                S.op("act", lambda e, tt_=tt_: e.activation(out=tt_[0:64, :], in_=tt_[0:64, :], func=AF.Sin, scale=2 * PI),
                     reads=[key], writes=[key])
                if sgn:
                    S.op("dve", lambda e, tt_=tt_: e.tensor_scalar(
                        out=tt_[0:64, :], in0=tt_[0:64, :], scalar1=self.cols[0:64, COL_SGN:COL_SGN + 1], scalar2=None,
                        op0=ALU.mult), reads=[key, "cols"], writes=[key])
                S.dma("pool", dst[:, c * CH:(c + 1) * CH], tt_[0:64, :], reads=[key],
                      writes=["ropeC" if dst is self.ropeC else "ropeS"])
        if ("rope",) in self.taps:
            o = self.outp("tap_ropeC", [64, SEQ])
            S.dma("sp", o[:, :], self.ropeC[:, :], reads=["ropeC"])
            o = self.outp("tap_ropeS", [64, SEQ])
            S.dma("sp", o[:, :], self.ropeS[:, :], reads=["ropeS"])
        S.barrier()
        ar.release()

    def gather_rows(self, name, src, src_key, rows, cols, dt, chunk_rows):
        S = self.S
        out = []
        for r0 in range(0, rows, chunk_rows):
            cr = min(chunk_rows, rows - r0)
            g = self.dram("%s_g%d" % (name, r0), [4 * cr, cols], dt)
            S.collective("AllGather", GROUPS4, src[r0:r0 + cr, :], g.ap().opt(), reads=[src_key], writes=[(name, "g")])
            out.append((g, r0, cr))
        return out

    def phase_G1(self, l):
        A = self.A_out
        self.GAg = self.gather_rows("ga%d" % l, A["ga"], "ga", self.GA_ROWS, NT, BF16, 256)
        self.GAfg = self.gather_rows("gaf%d" % l, A["gaf"], "gaf", 144, NT, F32, 128)
        self.GAtg = self.gather_rows("gat%d" % l, A["gat"], "gat", NT, 1792, BF16, 256)
        self.gb = self.dram("gb_src_%d" % l, [768, SEQ], BF16)

    def ga_ap(self, row0, nrows, rk, u0, n):
        j = row0 // 256
        g, r0, cr = self.GAg[j]
        o = row0 - r0
        return g[rk * cr + o:rk * cr + o + nrows, u0:u0 + n]

    def gaf_ap(self, row0, nrows, rk, u0, n):
        j = row0 // 128
        g, r0, cr = self.GAfg[j]
        o = row0 - r0
        return g[rk * cr + o:rk * cr + o + nrows, u0:u0 + n]

    def phase_MLA(self, l):
        S, ar, ps = self.S, self.ar, self.ps
        ar.mark()
        gb = self.gb
        wq_f = ar.alloc(6 * 1024)
        wq = ar.alloc(6 * 1024, BF16)
        wkv = ar.alloc(4 * 1024, BF16)
        wq3 = wq.rearrange("p (k c) -> p k c", k=6)
        wkv3 = wkv.rearrange("p (k c) -> p k c", k=4)
        wqf3 = wq_f.rearrange("p (k c) -> p k c", k=6)
        S.dma("sp", wqf3, self.wsh[("wuq_own", l)].ap().rearrange("(k p) n -> p k n", p=128), writes=["wq_f"])
        S.op("dve", lambda e: e.tensor_copy(out=wq, in_=wq_f), reads=["wq_f"], writes=["wq"])
        wkvf3 = wq_f[:, 0:4096].rearrange("p (k c) -> p k c", k=4)
        S.dma("sp", wkvf3, self.wsh[("wukv_own", l)].ap().rearrange("(k p) n -> p k n", p=128), reads=["wq"], writes=["wq_f"])
        S.op("dve", lambda e: e.tensor_copy(out=wkv, in_=wq_f[:, 0:4096]), reads=["wq_f"], writes=["wkv"])
        masks = ar.alloc(4 * 512, BF16)
        S.dma("sp", masks, self.mla_mask_in[:, :], writes=["masks"])
        masks3 = masks.rearrange("p (r c) -> p r c", r=4)

        QTn = ar.alloc(SEQ, BF16)
        QTr = ar.alloc(SEQ, BF16)
        KTn = ar.alloc(SEQ, BF16)
        KTr = ar.alloc(SEQ, BF16)
        V = ar.alloc(64 * 128, BF16)
        V3 = V.rearrange("p (t d) -> p t d", t=64)
        cqc = [ar.alloc(6 * 512, BF16) for _ in range(2)]
        ckc = [ar.alloc(4 * 512, BF16) for _ in range(2)]
        krc = [ar.alloc(2 * 512) for _ in range(2)]
        rc = [ar.alloc(2 * 512) for _ in range(2)]
        tmpa = ar.alloc(512)
        tmpb = ar.alloc(512)
        PT = [ar.alloc(512, BF16) for _ in range(3)]
        rec = ar.alloc(512)
        ost = [ar.alloc(512, BF16) for _ in range(2)]
        SCALE = 192.0 ** -0.5
        inrot, ptrot, orot = Rot(2), Rot(3), Rot(2)

        for hh in range(4):
            for tch in range(16):
                rk, u0 = tch // 4, 512 * (tch % 4)
                tcol = tch * 512
                si = inrot.next()
                cq3 = cqc[si].rearrange("p (k c) -> p k c", k=6)
                ck3 = ckc[si].rearrange("p (k c) -> p k c", k=4)
                for kc in range(6):
                    S.dma("sp", cq3[:, kc, :], self.ga_ap(self.GA_OFF["cq"] + kc * 128, 128, rk, u0, 512),
                          reads=[("ga%d" % l, "g")], writes=[("cqc", si)])
                for kc in range(4):
                    S.dma("sp", ck3[:, kc, :], self.ga_ap(self.GA_OFF["ckv"] + kc * 128, 128, rk, u0, 512),
                          reads=[("ga%d" % l, "g")], writes=[("ckc", si)])
                S.dma("sp", rc[si][0:64, 0:512], self.ropeC[:, tcol:tcol + 512], reads=["ropeC"], writes=[("rc", si)])
                S.dma("sp", rc[si][0:64, 512:1024], self.ropeS[:, tcol:tcol + 512], reads=["ropeS"], writes=[("rc", si)])
                if hh == 0:
                    S.dma("sp", krc[si][0:64, 0:512], self.gaf_ap(0, 64, rk, u0, 512), reads=[("gaf%d" % l, "g")],
                          writes=[("krc", si)])
                    S.dma("sp", krc[si][0:64, 512:1024], self.gaf_ap(64, 64, rk, u0, 512), reads=[("gaf%d" % l, "g")],
                          writes=[("krc", si)])
                    S.op("dve", lambda e, si=si: e.tensor_tensor(out=tmpa[0:64, :], in0=krc[si][0:64, 0:512],
                                                                 in1=rc[si][0:64, 0:512], op=ALU.mult),
                         reads=[("krc", si), ("rc", si)], writes=["tmpa"])
                    S.op("dve", lambda e, si=si: e.tensor_tensor(out=tmpb[0:64, :], in0=krc[si][0:64, 512:1024],
                                                                 in1=rc[si][0:64, 512:1024], op=ALU.mult),
                         reads=[("krc", si), ("rc", si)], writes=["tmpb"])
                    S.op("dve", lambda e, tcol=tcol: e.tensor_tensor(out=KTr[0:64, tcol:tcol + 512], in0=tmpa[0:64, :],
                                                                     in1=tmpb[0:64, :], op=ALU.add),
                         reads=["tmpa", "tmpb"], writes=[("KTr", tch)])
                cb = hh * 256
                for kc in range(6):
                    S.op("pe", lambda e, kc=kc, cq3=cq3, cb=cb: e.matmul(ps[0][:, :], lhsT=wq3[:, kc, cb:cb + 128], rhs=cq3[:, kc, :],
                                                                       start=(kc == 0), stop=(kc == 5)),
                         reads=["wq", ("cqc", si)], writes=[("ps", 0)])
                S.op("act", lambda e, tcol=tcol: e.activation(out=QTn[:, tcol:tcol + 512], in_=ps[0][:, :], func=AF.Copy),
                     reads=[("ps", 0)], writes=[("QTn", tch)])
                for (bank, c0) in ((1, cb + 128), (2, cb + 192)):
                    for kc in range(6):
                        S.op("pe", lambda e, kc=kc, cq3=cq3, bank=bank, c0=c0: e.matmul(
                            ps[bank][0:64, :], lhsT=wq3[:, kc, c0:c0 + 64], rhs=cq3[:, kc, :], start=(kc == 0), stop=(kc == 5)),
                            reads=["wq", ("cqc", si)], writes=[("ps", bank)])
                S.op("dve", lambda e, si=si: e.tensor_tensor(out=tmpa[0:64, :], in0=ps[1][0:64, :], in1=rc[si][0:64, 0:512],
                                                             op=ALU.mult), reads=[("ps", 1), ("rc", si)], writes=["tmpa"])
                S.op("dve", lambda e, si=si: e.tensor_tensor(out=tmpb[0:64, :], in0=ps[2][0:64, :], in1=rc[si][0:64, 512:1024],
                                                             op=ALU.mult), reads=[("ps", 2), ("rc", si)], writes=["tmpb"])
                S.op("dve", lambda e, tcol=tcol: e.tensor_tensor(out=QTr[0:64, tcol:tcol + 512], in0=tmpa[0:64, :],
                                                                 in1=tmpb[0:64, :], op=ALU.add),
                     reads=["tmpa", "tmpb"], writes=[("QTr", tch)])
                kb = hh * 256
                for kc in range(4):
                    S.op("pe", lambda e, kc=kc, ck3=ck3, kb=kb: e.matmul(ps[3][:, :], lhsT=wkv3[:, kc, kb:kb + 128], rhs=ck3[:, kc, :],
                                                                       start=(kc == 0), stop=(kc == 3)),
                         reads=["wkv", ("ckc", si)], writes=[("ps", 3)])
                S.op("act", lambda e, tcol=tcol: e.activation(out=KTn[:, tcol:tcol + 512], in_=ps[3][:, :], func=AF.Copy),
                     reads=[("ps", 3)], writes=[("KTn", tch)])
                for ts_ in range(4):
                    for kc in range(4):
                        S.op("pe", lambda e, kc=kc, ck3=ck3, kb=kb, ts_=ts_: e.matmul(
                            ps[4][:, ts_ * 128:(ts_ + 1) * 128], lhsT=ck3[:, kc, ts_ * 128:(ts_ + 1) * 128],
                            rhs=wkv3[:, kc, kb + 128:kb + 256], start=(kc == 0), stop=(kc == 3)),
                            reads=["wkv", ("ckc", si)], writes=[("ps", 4)])
                S.op("act", lambda e, tch=tch: e.activation(out=V[:, tch * 512:(tch + 1) * 512], in_=ps[4][:, :], func=AF.Copy),
                     reads=[("ps", 4)], writes=[("V", tch)])
            allk = lambda nm: [(nm, t) for t in range(16)]
            QTn4 = QTn.rearrange("p (r u) -> p r u", r=4)
            QTr4 = QTr.rearrange("p (r u) -> p r u", r=4)
            for I in range(16):
                qn = QTn4[:, :, I * 128:(I + 1) * 128]
                qr = QTr4[0:64, :, I * 128:(I + 1) * 128]
                tiles = [(rk_, ik_, None) for ik_ in range(I) for rk_ in range(4)] + [(rk_, I, rk_) for rk_ in range(4)]
                nt_ = len(tiles)
                def issue_S(ti):
                    rk_, ik_, mk = tiles[ti]
                    kcol = rk_ * 2048 + ik_ * 128
                    sb_ = 5 + (ti % 2)
                    S.op("pe", lambda e, kcol=kcol, sb_=sb_, qn=qn: e.matmul(ps[sb_][:, :], lhsT=KTn[:, kcol:kcol + 128], rhs=qn,
                                                                           start=True, stop=False),
                         reads=allk("KTn") + allk("QTn"), writes=[("ps", sb_)])
                    S.op("pe", lambda e, kcol=kcol, sb_=sb_, qr=qr: e.matmul(ps[sb_][:, :], lhsT=KTr[0:64, kcol:kcol + 128], rhs=qr,
                                                                           start=False, stop=True),
                         reads=allk("KTr") + allk("QTr"), writes=[("ps", sb_)])

                def issue_rest(ti):
                    rk_, ik_, mk = tiles[ti]
                    kcol = rk_ * 2048 + ik_ * 128
                    vt = kcol // 128
                    sb_ = 5 + (ti % 2)
                    pi = ptrot.next()
                    S.op("act", lambda e, pi=pi, sb_=sb_: e.activation(out=PT[pi], in_=ps[sb_][:, :], func=AF.Exp, scale=SCALE),
                         reads=[("ps", sb_)], writes=[("PT", pi)])
                    if mk is not None:
                        S.op("pool", lambda e, pi=pi, mk=mk: e.tensor_tensor(out=PT[pi], in0=PT[pi], in1=masks3[:, mk, :],
                                                                             op=ALU.mult),
                             reads=[("PT", pi), "masks"], writes=[("PT", pi)])
                    S.op("pe", lambda e, pi=pi, vt=vt, ti=ti, nt_=nt_: e.matmul(ps[0][:, :], lhsT=V3[:, vt, :], rhs=PT[pi],
                                                                               start=(ti == 0), stop=(ti == nt_ - 1)),
                         reads=[("PT", pi)] + allk("V"), writes=[("ps", 0)])
                    S.op("pe", lambda e, pi=pi, ti=ti, nt_=nt_: e.matmul(ps[1][:, :], lhsT=self.ones_b, rhs=PT[pi],
                                                                        start=(ti == 0), stop=(ti == nt_ - 1)),
                         reads=[("PT", pi), "ones_b"], writes=[("ps", 1)])

                issue_S(0)
                if nt_ > 1:
                    issue_S(1)
                for ti in range(nt_):
                    issue_rest(ti)
                    if ti + 2 < nt_:
                        issue_S(ti + 2)
                S.op("dve", lambda e: e.reciprocal(out=rec, in_=ps[1][:, :]), reads=[("ps", 1)], writes=["rec"])
                oi = orot.next()
                S.op("dve", lambda e, oi=oi: e.tensor_tensor(out=ost[oi], in0=ps[0][:, :], in1=rec, op=ALU.mult),
                     reads=[("ps", 0), "rec"], writes=[("ost", oi)])
                gbv = gb.ap().rearrange("c (r u) -> c r u", r=4)
                S.dma("pool", gbv[hh * 128:(hh + 1) * 128, :, I * 128:(I + 1) * 128],
                      ost[oi].rearrange("p (r q) -> p r q", r=4), reads=[("ost", oi)], writes=["gb"])
        if ("gb", l) in self.taps:
            o = self.outp("tap_gb_%d" % l, [512, SEQ], BF16)
            for r0 in range(0, 512, 128):
                S.dma("sp", o[r0:r0 + 128, :], gb[r0:r0 + 128, :], reads=["gb"])
        S.barrier()
        ar.release()


    def phase_DSA(self, l):
        S, ar, ps = self.S, self.ar, self.ps
        ar.mark()
        odT = self.dram("odsaT_%d" % l, [1024, NT], BF16)
        self.odT = odT
        A = self.A_out
        gkey = ("ga%d" % l, "g")
        dk = ar.alloc(2 * SEQ, BF16)
        dk3 = dk.rearrange("p (g t) -> p g t", g=2)
        ik = ar.alloc(SEQ, BF16)
        ik4 = ik.rearrange("p (r u) -> p r u", r=4)
        dvt = ar.alloc(64 * 256, BF16)
        dvt3 = dvt.rearrange("p (t c) -> p t c", t=64)
        for g_ in range(2):
            for rk in range(4):
                S.dma("sp", dk3[:, g_, rk * 2048:(rk + 1) * 2048], self.ga_ap(self.GA_OFF["dk"] + g_ * 128, 128, rk, 0, 2048),
                      reads=[gkey], writes=["dk"])
        for rk in range(4):
            S.dma("sp", ik[0:64, rk * 2048:(rk + 1) * 2048], self.ga_ap(self.GA_OFF["ik"], 64, rk, 0, 2048), reads=[gkey],
                  writes=["ik"])
        for rk in range(4):
            for i in range(16):
                g, r0, cr = self.GAtg[i // 2]
                o = (i % 2) * 128
                S.dma("sp", dvt3[:, rk * 16 + i, :], g[rk * cr + o:rk * cr + o + 128, 1536:1792],
                      reads=[("gat%d" % l, "g")], writes=["dvt"])
        dmask = ar.alloc(512)
        S.dma("sp", dmask, self.dsa_dmask_in[:, :], writes=["dmask"])
        c01 = ar.alloc(512, BF16)
        S.dma("sp", c01, self.dsa_c01_in[:, :], writes=["c01"])
        iq_s = ar.alloc(32 * 128, BF16)
        iq3 = iq_s.rearrange("p (h q) -> p h q", h=32)
        iw_s = ar.alloc(32)
        dq_s = ar.alloc(8 * 128, BF16)
        dq3 = dq_s.rearrange("p (h q) -> p h q", h=8)
        Dg = ar.alloc(32 * 128, BF16)
        Dg3 = Dg.rearrange("p (h q) -> p h q", h=32)
        w = ar.alloc(SEQ)
        m8 = ar.alloc(8)
        maskq = ar.alloc(SEQ, BF16)
        maskT = ar.alloc(16 * 512, BF16)
        maskT3 = maskT.rearrange("p (k c) -> p k c", k=16)
        Ah = [ar.alloc(512, BF16) for _ in range(4)]
        PT = [ar.alloc(512, BF16) for _ in range(3)]
        rec = ar.alloc(512)
        ost = [ar.alloc(512, BF16) for _ in range(2)]
        arot, ptrot, orot = Rot(4), Rot(3), Rot(2)
        SCALE = 128.0 ** -0.5
        IMM = -2.0e30
        ps7b = ps[7][:, :].bitcast(BF16)
        iqv = A["iqT"].ap().rearrange("(h d) u -> d h u", d=64)
        dqv = A["dqT"].ap().rearrange("(h d) u -> d h u", d=128)
        odv = odT.ap().rearrange("(h d) u -> d h u", d=128)
        dq_s2 = ar.alloc(8 * 128, BF16)
        dq3l = [dq3, dq_s2.rearrange("p (h q) -> p h q", h=8)]

        def st_load_idx(i):
                L = (i + 1) * 512
                q0 = i * 128
                S.dma("sp", iq3[0:64, :, :], iqv[:, :, q0:q0 + 128], reads=["iqT"], writes=["iq_s"])
                S.dma("sp", iw_s, A["iw"][q0:q0 + 128, :], reads=["iw"], writes=["iw_s"])
                S.dma("sp", dq3l[i % 2], dqv[:, :, q0:q0 + 128], reads=["dqT"], writes=[("dq_s", i % 2)])
                for h in range(32):
                    S.op("dve", lambda e, h=h: e.tensor_scalar(out=Dg3[:, h, :], in0=self.ident_b, scalar1=iw_s[:, h:h + 1],
                                                              scalar2=None, op0=ALU.mult),
                         reads=["ident_b", "iw_s"], writes=["Dg"])
                for kg in range(i + 1):
                    kc_ = ik4[0:64, :, kg * 128:(kg + 1) * 128]
                    accb = 4

                    def idx_S(h, kc_=kc_):
                        sb_ = 2 + (h % 2)
                        S.op("pe", lambda e, h=h, sb_=sb_, kc_=kc_: e.matmul(ps[sb_][:, :], lhsT=iq3[0:64, h, :], rhs=kc_,
                                                                           start=True, stop=True),
                             reads=["iq_s", "ik"], writes=[("ps", sb_)])

                    idx_S(0)
                    idx_S(1)
                    for h in range(32):
                        sb_ = 2 + (h % 2)
                        ai = arot.next()
                        if h % 2 == 0:
                            S.op("act", lambda e, ai=ai, sb_=sb_: e.activation(out=Ah[ai], in_=ps[sb_][:, :], func=AF.Relu),
                                 reads=[("ps", sb_)], writes=[("Ah", ai)])
                        else:
                            S.op("dve", lambda e, ai=ai, sb_=sb_: e.tensor_scalar(out=Ah[ai], in0=ps[sb_][:, :], scalar1=0.0,
                                                                               scalar2=None, op0=ALU.max),
                                 reads=[("ps", sb_)], writes=[("Ah", ai)])
                        S.op("pe", lambda e, h=h, ai=ai, accb=accb: e.matmul(ps[accb][:, :], lhsT=Dg3[:, h, :], rhs=Ah[ai],
                                                                            start=(h == 0), stop=(h == 31)),
                             reads=["Dg", ("Ah", ai)], writes=[("ps", accb)])
                        if h + 2 < 32:
                            idx_S(h + 2)
                    if kg < i:
                        S.op("act", lambda e, kg=kg, accb=accb: e.activation(out=w[:, kg * 512:(kg + 1) * 512], in_=ps[accb][:, :], func=AF.Copy),
                             reads=[("ps", accb)], writes=["w"])
                    else:
                        S.op("dve", lambda e, kg=kg, accb=accb: e.tensor_tensor(out=w[:, kg * 512:(kg + 1) * 512], in0=ps[accb][:, :], in1=dmask,
                                                                               op=ALU.add), reads=[("ps", accb), "dmask"], writes=["w"])

        def st_topk(i):
                L = (i + 1) * 512
                for rd in range(32):
                    S.op("dve", lambda e, L=L: e.max(out=m8, in_=w[:, 0:L]), reads=["w"], writes=["m8"])
                    S.op("dve", lambda e, L=L: e.match_replace(out=w[:, 0:L], in_to_replace=m8, in_values=w[:, 0:L], imm_value=IMM),
                         reads=["w", "m8"], writes=["w"])
                S.op("dve", lambda e, L=L: e.tensor_scalar(out=maskq[:, 0:L], in0=w[:, 0:L], scalar1=-1.5e30, scalar2=None,
                                                          op0=ALU.is_lt), reads=["w"], writes=["maskq"])
                S.op("dve", lambda e, i=i: e.tensor_tensor(out=maskq[:, i * 512:(i + 1) * 512], in0=maskq[:, i * 512:(i + 1) * 512],
                                                          in1=c01, op=ALU.mult), reads=["maskq", "c01"], writes=["maskq"])

        def st_T(i):
                for kg in range(i + 1):
                    for rk in range(4):
                        S.op("pe", lambda e, kg=kg, rk=rk: e.transpose(out=ps7b[:, rk * 128:(rk + 1) * 128],
                                                                      in_=maskq[:, kg * 512 + rk * 128:kg * 512 + (rk + 1) * 128],
                                                                      identity=self.ident_b),
                             reads=["maskq", "ident_b"], writes=[("ps", 7)])
                    S.op("act", lambda e, kg=kg: e.activation(out=maskT3[:, kg, :], in_=ps7b[:, 0:512], func=AF.Copy),
                         reads=[("ps", 7)], writes=[("maskT", kg)])

        def st_att(i):
                q0 = i * 128
                for g_ in range(2):
                    nt_ = 4 * (i + 1)
                    tl = [(kg, rk) for kg in range(i + 1) for rk in range(4)]

                    def att_S(ti, g_=g_):
                        kg, rk = tl[ti]
                        col = rk * 2048 + kg * 128
                        sb_ = 5 + (ti % 2)
                        S.op("pe", lambda e, col=col, sb_=sb_, g_=g_: e.matmul(ps[sb_][:, :], lhsT=dk3[:, g_, col:col + 128],
                                                                             rhs=dq3l[i % 2][:, 4 * g_:4 * g_ + 4, :], start=True, stop=True),
                             reads=["dk", ("dq_s", i % 2)], writes=[("ps", sb_)])

                    def att_rest(ti, g_=g_, nt_=nt_):
                        kg, rk = tl[ti]
                        tile_ = rk * 16 + kg
                        sb_ = 5 + (ti % 2)
                        pi = ptrot.next()
                        S.op("act", lambda e, pi=pi, sb_=sb_: e.activation(out=PT[pi], in_=ps[sb_][:, :], func=AF.Exp, scale=SCALE),
                             reads=[("ps", sb_)], writes=[("PT", pi)])
                        for hq in range(4):
                            S.op("pool", lambda e, pi=pi, kg=kg, rk=rk, hq=hq: e.tensor_tensor(
                                out=PT[pi][:, hq * 128:(hq + 1) * 128], in0=PT[pi][:, hq * 128:(hq + 1) * 128],
                                in1=maskT3[:, kg, rk * 128:(rk + 1) * 128], op=ALU.mult),
                                reads=[("PT", pi), ("maskT", kg)], writes=[("PT", pi)])
                        S.op("pe", lambda e, pi=pi, tile_=tile_, ti=ti, nt_=nt_, g_=g_: e.matmul(
                            ps[2 * g_][:, :], lhsT=dvt3[:, tile_, g_ * 128:(g_ + 1) * 128], rhs=PT[pi], start=(ti == 0), stop=(ti == nt_ - 1)),
                            reads=[("PT", pi), "dvt"], writes=[("ps", 2 * g_)])
                        S.op("pe", lambda e, pi=pi, ti=ti, nt_=nt_, g_=g_: e.matmul(ps[2 * g_ + 1][:, :], lhsT=self.ones_b, rhs=PT[pi],
                                                                            start=(ti == 0), stop=(ti == nt_ - 1)),
                             reads=[("PT", pi), "ones_b"], writes=[("ps", 2 * g_ + 1)])

                    att_S(0)
                    att_S(1)
                    for ti in range(nt_):
                        att_rest(ti)
                        if ti + 2 < nt_:
                            att_S(ti + 2)
                    S.op("dve", lambda e, g_=g_: e.reciprocal(out=rec, in_=ps[2 * g_ + 1][:, :]), reads=[("ps", 2 * g_ + 1)], writes=["rec"])
                    oi = orot.next()
                    S.op("dve", lambda e, oi=oi, g_=g_: e.tensor_tensor(out=ost[oi], in0=ps[2 * g_][:, :], in1=rec, op=ALU.mult),
                         reads=[("ps", 2 * g_), "rec"], writes=[("ost", oi)])
                    S.dma("pool", odv[:, 4 * g_:4 * g_ + 4, q0:q0 + 128], ost[oi].rearrange("p (h q) -> p h q", h=4),
                          reads=[("ost", oi)], writes=["odT"])

        for i in range(16):
            st_load_idx(i)
            st_topk(i)
            if i > 0:
                st_att(i - 1)
            st_T(i)
        st_att(15)
        if ("odT", l) in self.taps:
            o = self.outp("tap_odT_%d" % l, [1024, NT], BF16)
            for r0 in range(0, 1024, 128):
                S.dma("sp", o[r0:r0 + 128, :], odT[r0:r0 + 128, :], reads=["odT"])
        S.barrier()
        ar.release()


    def phase_GLA(self, l):
        S, ar, ps = self.S, self.ar, self.ps
        ar.mark()
        gkey = ("ga%d" % l, "g")
        gb = self.gb
        qk_all = self.dram("glaqk_all_%d" % l, [4 * 256, SEQ], BF16)
        qk_own = self.dram("glaqk_own_%d" % l, [256, SEQ], BF16)
        tok_own = self.dram("glatok_own_%d" % l, [SEQ, 384], BF16)
        for hd in range(4):
            for wi, nm in enumerate(("gq", "gk")):
                for rk in range(4):
                    S.dma("sp", qk_all[hd * 256 + wi * 128:hd * 256 + (wi + 1) * 128, rk * 2048:(rk + 1) * 2048],
                          self.ga_ap(self.GA_OFF[nm] + hd * 128, 128, rk, 0, 2048), reads=[gkey], writes=["qk_all"])

        def hd_of(e):
            return self.rank_of(e)

        for c4 in range(4):
            S.dma("pool", qk_own[:, c4 * 2048:(c4 + 1) * 2048],
                  lambda e, c4=c4: qk_all[bass.ds(hd_of(e) * 256, 256), c4 * 2048:(c4 + 1) * 2048],
                  reads=["qk_all"], writes=["qk_own"])
        for rk in range(4):
            for j in range(8):
                g, r0, cr = self.GAtg[j]
                rows = slice(rk * cr, (rk + 1) * cr)
                t0 = rk * 2048 + j * 256
                S.dma("pool", tok_own[t0:t0 + 256, 0:128],
                      lambda e, g=g, rows=rows: g[rows, bass.ds(hd_of(e) * 128, 128)],
                      reads=[("gat%d" % l, "g")], writes=["tok_own"])
                S.dma("pool", tok_own[t0:t0 + 256, 128:384],
                      lambda e, g=g, rows=rows: g[rows, bass.ds(512 + hd_of(e) * 256, 256)],
                      reads=[("gat%d" % l, "g")], writes=["tok_own"])
        U = ar.alloc(64)
        Lm = ar.alloc(64)
        tri = ar.alloc(64)
        S.dma("sp", U[0:64, :], self.gla_consts_in[0:64, 0:64], writes=["U"])
        S.dma("sp", Lm[0:64, :], self.gla_consts_in[0:64, 64:128], writes=["Lm"])
        S.dma("sp", tri[0:64, :], self.gla_consts_in[0:64, 128:192], writes=["tri"])
        wg2 = ar.alloc(128)
        bg = ar.alloc(128)
        S.dma("sp", wg2[0:16, :], self.gla_wg2_in[l][0:16, :], writes=["wg2"])
        S.dma("sp", bg[0:1, :], self.gla_wg2_in[l][16:17, :], writes=["bgr"])
        gout = ar.alloc(256)
        S.dma("sp", gout[0:64, :], self.gla_gout_in[l][0:1, :].partition_broadcast(64), writes=["gout"])
        state = ar.alloc(256)
        state_b = ar.alloc(256, BF16)
        S.op("dve", lambda e: e.memset(state, 0.0), writes=["state"])
        S.op("dve", lambda e: e.memset(state_b, 0.0), writes=["state_b"])
        NS = 2
        glw = [ar.alloc(64) for _ in range(NS)]
        qT = [ar.alloc(64, BF16) for _ in range(NS)]
        kT = [ar.alloc(64, BF16) for _ in range(NS)]
        tk = [ar.alloc(384, BF16) for _ in range(NS)]
        sp_ = ar.alloc(128)
        E1 = ar.alloc(64)
        E2 = ar.alloc(64)
        E3 = ar.alloc(128)
        qd = ar.alloc(64, BF16)
        kinv = ar.alloc(64, BF16)
        kend = ar.alloc(128, BF16)
        attT = ar.alloc(64, BF16)
        junk = ar.alloc(256)
        ssq = ar.alloc(4)
        on = ar.alloc(256, BF16)
        ostg = [ar.alloc(128, BF16) for _ in range(2)]
        ps7b = ps[7][:, :].bitcast(BF16)
        rot, orot = Rot(NS), Rot(2)
        DKS = 128.0 ** -0.5
        gfkey = ("gaf%d" % l, "g")
        gfg, gfr0, gfcr = self.GAfg[1]
        for n in range(128):
            j = n // 2
            rk, i = j % 4, j // 4
            u0 = i * 128 + (n % 2) * 64
            tau0 = rk * 2048 + u0
            s_ = rot.next()
            S.dma("sp", glw[s_][0:16, :], gfg[rk * gfcr:rk * gfcr + 16, u0:u0 + 64], reads=[gfkey], writes=[("glw", s_)])
            S.dma("sp", qT[s_], qk_own[0:128, tau0:tau0 + 64], reads=["qk_own"], writes=[("qT", s_)])
            S.dma("sp", kT[s_], qk_own[128:256, tau0:tau0 + 64], reads=["qk_own"], writes=[("kT", s_)])
            S.dma("sp", tk[s_][0:64, :], tok_own[tau0:tau0 + 64, :], reads=["tok_own"], writes=[("tk", s_)])
            S.op("pe", lambda e, s_=s_: e.matmul(ps[2][0:64, 0:128], lhsT=glw[s_][0:16, :], rhs=wg2[0:16, :], start=True, stop=False),
                 reads=[("glw", s_), "wg2"], writes=[("ps", 2)])
            S.op("pe", lambda e: e.matmul(ps[2][0:64, 0:128], lhsT=self.ones_f[0:1, 0:64], rhs=bg[0:1, :], start=False, stop=True),
                 reads=["ones_f", "bgr"], writes=[("ps", 2)])
            S.op("act", lambda e: e.activation(out=sp_[0:64, :], in_=ps[2][0:64, 0:128], func=AF.Exp, scale=-1.0),
                 reads=[("ps", 2)], writes=["sp"])
            S.op("act", lambda e: e.activation(out=sp_[0:64, :], in_=sp_[0:64, :], func=AF.Ln, bias=1.0), reads=["sp"], writes=["sp"])
            S.op("pe", lambda e: e.matmul(ps[3][:, 0:64], lhsT=sp_[0:64, :], rhs=U[0:64, :], start=True, stop=True),
                 reads=["sp", "U"], writes=[("ps", 3)])
            S.op("pe", lambda e: e.matmul(ps[4][0:64, 0:128], lhsT=Lm[0:64, :], rhs=sp_[0:64, :], start=True, stop=True),
                 reads=["sp", "Lm"], writes=[("ps", 4)])
            S.op("act", lambda e: e.activation(out=E1, in_=ps[3][:, 0:64], func=AF.Exp), reads=[("ps", 3)], writes=["E1"])
            S.op("act", lambda e: e.activation(out=E2, in_=ps[3][:, 0:64], func=AF.Exp, scale=-1.0), reads=[("ps", 3)], writes=["E2"])
            S.op("act", lambda e: e.activation(out=E3[0:64, :], in_=ps[4][0:64, 0:128], func=AF.Exp), reads=[("ps", 4)], writes=["E3"])
            S.op("dve", lambda e, s_=s_: e.scalar_tensor_tensor(out=qd, in0=qT[s_], scalar=DKS, in1=E1, op0=ALU.mult, op1=ALU.mult),
                 reads=[("qT", s_), "E1"], writes=["qd"])
            S.op("dve", lambda e, s_=s_: e.tensor_tensor(out=kinv, in0=kT[s_], in1=E2, op=ALU.mult),
                 reads=[("kT", s_), "E2"], writes=["kinv"])
            S.op("dve", lambda e, s_=s_: e.tensor_tensor(out=kend[0:64, :], in0=tk[s_][0:64, 0:128], in1=E3[0:64, :], op=ALU.mult),
                 reads=[("tk", s_), "E3"], writes=["kend"])
            S.op("pe", lambda e: e.matmul(ps[5][0:64, 0:64], lhsT=kinv, rhs=qd, start=True, stop=True),
                 reads=["kinv", "qd"], writes=[("ps", 5)])
            S.op("dve", lambda e: e.tensor_tensor(out=attT[0:64, :], in0=ps[5][0:64, 0:64], in1=tri[0:64, :], op=ALU.mult),
                 reads=[("ps", 5), "tri"], writes=["attT"])
            S.op("pe", lambda e, s_=s_: e.matmul(ps[0][0:64, 0:256], lhsT=attT[0:64, :], rhs=tk[s_][0:64, 128:384], start=True, stop=False),
                 reads=["attT", ("tk", s_)], writes=[("ps", 0)])
            S.op("pe", lambda e: e.matmul(ps[0][0:64, 0:256], lhsT=qd, rhs=state_b, start=False, stop=True),
                 reads=["qd", "state_b"], writes=[("ps", 0)])
            S.op("pe", lambda e, s_=s_: e.matmul(ps[1][:, 0:256], lhsT=kend[0:64, :], rhs=tk[s_][0:64, 128:384], start=True, stop=True),
                 reads=["kend", ("tk", s_)], writes=[("ps", 1)])
            S.op("dve", lambda e: e.scalar_tensor_tensor(out=state, in0=state, scalar=E1[:, 63:64], in1=ps[1][:, 0:256],
                                                         op0=ALU.mult, op1=ALU.add),
                 reads=["state", "E1", ("ps", 1)], writes=["state"])
            S.op("dve", lambda e: e.tensor_copy(out=state_b, in_=state), reads=["state"], writes=["state_b"])
            S.op("act", lambda e: e.activation(out=junk[0:64, :], in_=ps[0][0:64, 0:256], func=AF.Square, accum_out=ssq[0:64, 0:1]),
                 reads=[("ps", 0)], writes=["junk", "ssq0"])
            S.op("act", lambda e: e.activation(out=ssq[0:64, 1:2], in_=ssq[0:64, 0:1], func=AF.Sqrt, scale=1.0 / 256, bias=EPS),
                 reads=["ssq0"], writes=["ssq1"])
            S.op("dve", lambda e: e.reciprocal(out=ssq[0:64, 2:3], in_=ssq[0:64, 1:2]), reads=["ssq1"], writes=["ssq2"])
            S.op("dve", lambda e: e.scalar_tensor_tensor(out=on[0:64, :], in0=ps[0][0:64, 0:256], scalar=ssq[0:64, 2:3],
                                                         in1=gout[0:64, :], op0=ALU.mult, op1=ALU.mult),
                 reads=[("ps", 0), "ssq2", "gout"], writes=["on"])
            for c2 in range(2):
                S.op("pe", lambda e, c2=c2: e.transpose(out=ps7b[:, c2 * 64:(c2 + 1) * 64], in_=on[0:64, c2 * 128:(c2 + 1) * 128],
                                                        identity=self.ident_b[0:64, 0:64]),
                     reads=["on", "ident_b"], writes=[("ps", 7)])
            oi = orot.next()
            S.op("act", lambda e, oi=oi: e.activation(out=ostg[oi], in_=ps7b[:, 0:128], func=AF.Copy),
                 reads=[("ps", 7)], writes=[("ostg", oi)])
            for c2 in range(2):
                S.dma("pool", gb[512 + c2 * 128:512 + (c2 + 1) * 128, tau0:tau0 + 64], ostg[oi][:, c2 * 64:(c2 + 1) * 64],
                      reads=[("ostg", oi)], writes=["gb"])
        if ("gbg", l) in self.taps:
            o = self.outp("tap_gbg_%d" % l, [256, SEQ], BF16)
            for r0 in range(0, 256, 128):
                S.dma("sp", o[r0:r0 + 128, :], gb[512 + r0:512 + r0 + 128, :], reads=["gb"])
        S.barrier()
        ar.release()

    def phase_G2(self, l):
        S = self.S
        self.GBg = self.gather_rows("gb%d" % l, self.gb, "gb", 768, SEQ, BF16, 64)
        yown = self.dram("yown_%d" % l, [4 * 768, NT], BF16)
        self.yown = yown

        def col0(e):
            if not hasattr(self, "_c0"):
                self._c0 = {}
            if id(e) not in self._c0:
                self._c0[id(e)] = self.rank_of(e) * 2048
            return self._c0[id(e)]

        for (g, r0, cr) in self.GBg:
            for rr in range(4):
                S.dma("pool", yown[rr * 768 + r0:rr * 768 + r0 + cr, :],
                      lambda e, g=g, rr=rr, cr=cr: g[rr * cr:(rr + 1) * cr, bass.ds(col0(e), 2048)],
                      reads=[("gb%d" % l, "g")], writes=["yown"])

    def phase_C(self, l, last):
        S, ar, ps = self.S, self.ar, self.ps
        ar.mark()
        A = self.A_out
        gbkey = ("gb%d" % l, "g")
        xsrc = self.x_own if l == 0 else self.dr["xres%d" % (l - 1)]
        if last:
            xdst = self.out_own
        else:
            xdst = self.dram("xres%d" % l, [NT, D], F32)
        yT = ar.alloc(32 * 512, BF16)
        yT3 = yT.rearrange("p (k t) -> p k t", k=32)
        mT = ar.alloc(32 * 512, BF16)
        mT3 = mT.rearrange("p (k t) -> p k t", k=32)
        wsl = [ar.alloc(32 * 512, BF16) for _ in range(2)]
        gate_bc = ar.alloc(D)
        S.dma("sp", gate_bc, self.dr["gatebc_%d" % l][:, :], reads=["gatebc_dram"], writes=["gate_bc"])
        if last:
            fg = ar.alloc(D)
            S.dma("sp", fg, self.final_g_in[0:1, :].partition_broadcast(128), writes=["fg"])
            xn = ar.alloc(D)
            junk = yT[:, 0:D]
            ss = ar.alloc(4)
        zt = [ar.alloc(512, BF16) for _ in range(2)]
        gt = [ar.alloc(3 * 512, BF16) for _ in range(2)]
        t1 = ar.alloc(512)
        t2 = ar.alloc(512)
        xc = [ar.alloc(512) for _ in range(2)]
        xo = [ar.alloc(512) for _ in range(2)]
        wrot, zrot, grot, xrot, brot = Rot(2), Rot(2), Rot(2), Rot(2), Rot(2)

        def my_rank(e):
            return self.rank_of(e)

        for tc in range(4):
            t0 = tc * 512
            for kc in range(32):
                if kc < 16:
                    rr, hh = kc // 4, kc % 4
                    S.dma("sp", yT3[:, kc, :], self.yown[rr * 768 + hh * 128:rr * 768 + (hh + 1) * 128, t0:t0 + 512],
                          reads=["yown"], writes=[("yT", kc)])
                elif kc < 24:
                    rr, c2 = (kc - 16) // 2, (kc - 16) % 2
                    S.dma("sp", yT3[:, kc, :], self.yown[rr * 768 + 512 + c2 * 128:rr * 768 + 512 + (c2 + 1) * 128, t0:t0 + 512],
                          reads=["yown"], writes=[("yT", kc)])
                else:
                    S.dma("sp", yT3[:, kc, :], self.odT[(kc - 24) * 128:(kc - 23) * 128, t0:t0 + 512], reads=["odT"],
                          writes=[("yT", kc)])
                zs = zrot.next()
                S.dma("sp", zt[zs], A["zT"][kc * 128:(kc + 1) * 128, t0:t0 + 512], reads=["zT"], writes=[("zt", zs)])
                S.op("pool", lambda e, kc=kc, zs=zs: e.tensor_tensor(out=yT3[:, kc, :], in0=yT3[:, kc, :], in1=zt[zs], op=ALU.mult),
                     reads=[("yT", kc), ("zt", zs)], writes=[("yT", kc)])
            yk = [("yT", kc) for kc in range(32)]
            for u in range(8):
                s_ = wrot.next()
                wbm = wsl[s_][:, 0:16 * 512].rearrange("p (k c) -> p k c", k=16)
                wbg = wsl[s_][:, 16 * 512:24 * 512].rearrange("p (k c) -> p k c", k=8)
                wbd = wsl[s_][:, 24 * 512:32 * 512].rearrange("p (k c) -> p k c", k=8)
                wkey = ("wsl", s_)
                self.load_unit("sp", wbm, "w_bm", l, u, 512, writes=[wkey])
                self.load_unit("sp", wbg, "w_bg", l, u, 512, writes=[wkey])
                self.load_unit("sp", wbd, "w_bd", l, u, 512, writes=[wkey])
                for pc in range(4):
                    dc = u * 4 + pc
                    gs = grot.next()
                    g3 = gt[gs].rearrange("p (b t) -> p b t", b=3)
                    for b_ in range(3):
                        S.dma("sp", g3[:, b_, :], A["gT"][b_ * D + dc * 128:b_ * D + (dc + 1) * 128, t0:t0 + 512], reads=["gT"],
                              writes=[("gt", gs)])
                    for (bank, w3, k0, nk) in ((0, wbm, 0, 16), (1, wbg, 16, 8), (2, wbd, 24, 8)):
                        for kk in range(nk):
                            S.op("pe", lambda e, bank=bank, w3=w3, kk=kk, k0=k0, nk=nk, pc=pc: e.matmul(
                                ps[bank][:, :], lhsT=w3[:, kk, pc * 128:(pc + 1) * 128], rhs=yT3[:, k0 + kk, :],
                                start=(kk == 0), stop=(kk == nk - 1)), reads=[wkey] + yk, writes=[("ps", bank)])
                    S.op("dve", lambda e, g3=g3: e.tensor_tensor(out=t1, in0=ps[0][:, :], in1=g3[:, 0, :], op=ALU.mult),
                         reads=[("ps", 0), ("gt", gs)], writes=["t1"])
                    S.op("dve", lambda e, g3=g3: e.tensor_tensor(out=t2, in0=ps[1][:, :], in1=g3[:, 1, :], op=ALU.mult),
                         reads=[("ps", 1), ("gt", gs)], writes=["t2"])
                    S.op("dve", lambda e: e.tensor_tensor(out=t1, in0=t1, in1=t2, op=ALU.add), reads=["t1", "t2"], writes=["t1"])
                    S.op("dve", lambda e, g3=g3: e.tensor_tensor(out=t2, in0=ps[2][:, :], in1=g3[:, 2, :], op=ALU.mult),
                         reads=[("ps", 2), ("gt", gs)], writes=["t2"])
                    S.op("dve", lambda e, dc=dc: e.tensor_tensor(out=mT3[:, dc, :], in0=t1, in1=t2, op=ALU.add),
                         reads=["t1", "t2"], writes=[("mT", dc)])
            mk = [("mT", dc) for dc in range(32)]
            for oc in range(8):
                s_ = wrot.next()
                wo = wsl[s_].rearrange("p (k c) -> p k c", k=32)
                wkey = ("wsl", s_)
                self.load_unit("sp", wo, "w_o", l, oc, 512, writes=[wkey])
                for tt in range(4):
                    u0 = t0 + tt * 128
                    bank = 3 + brot.next()
                    for dc in range(32):
                        S.op("pe", lambda e, dc=dc, tt=tt, bank=bank, wo=wo: e.matmul(
                            ps[bank][:, :], lhsT=mT3[:, dc, tt * 128:(tt + 1) * 128], rhs=wo[:, dc, :],
                            start=(dc == 0), stop=(dc == 31)), reads=[wkey] + mk, writes=[("ps", bank)])
                    xs = xrot.next()
                    S.dma("sp", xc[xs], xsrc[u0:u0 + 128, oc * 512:(oc + 1) * 512], writes=[("xc", xs)])
                    S.op("dve", lambda e, bank=bank, oc=oc: e.tensor_tensor(out=t1, in0=ps[bank][:, :],
                                                                          in1=gate_bc[:, oc * 512:(oc + 1) * 512], op=ALU.mult),
                         reads=[("ps", bank), "gate_bc"], writes=["t1"])
                    S.op("pool", lambda e, xs=xs: e.tensor_tensor(out=xo[xs], in0=t1, in1=xc[xs], op=ALU.add),
                         reads=["t1", ("xc", xs)], writes=[("xo", xs)])
                    S.dma("pool", self.xmid[u0:u0 + 128, oc * 512:(oc + 1) * 512] if last else xdst[u0:u0 + 128, oc * 512:(oc + 1) * 512],
                          xo[xs], reads=[("xo", xs)], writes=["xout"])
            if last:
                for tt in range(4):
                    u0 = t0 + tt * 128
                    for hh in range(2):
                        S.dma("sp", xn[:, hh * 2048:(hh + 1) * 2048], self.xmid[u0:u0 + 128, hh * 2048:(hh + 1) * 2048],
                              reads=["xout"], writes=["xn"])
                    S.op("act", lambda e: e.activation(out=junk, in_=xn, func=AF.Square, accum_out=ss[:, 0:1]),
                         reads=["xn"], writes=[("yT", kc_) for kc_ in range(8)] + ["ss0"])
                    S.op("act", lambda e: e.activation(out=ss[:, 1:2], in_=ss[:, 0:1], func=AF.Sqrt, scale=1.0 / D, bias=EPS),
                         reads=["ss0"], writes=["ss1"])
                    S.op("dve", lambda e: e.reciprocal(out=ss[:, 2:3], in_=ss[:, 1:2]), reads=["ss1"], writes=["ss2"])
                    S.op("dve", lambda e: e.scalar_tensor_tensor(out=xn, in0=xn, scalar=ss[:, 2:3], in1=fg, op0=ALU.mult, op1=ALU.mult),
                         reads=["xn", "ss2", "fg"], writes=["xn"])
                    for hh in range(2):
                        S.dma("pool", self.out_own[u0:u0 + 128, hh * 2048:(hh + 1) * 2048], xn[:, hh * 2048:(hh + 1) * 2048],
                              reads=["xn"], writes=["final"])
        if ("x1", l) in self.taps:
            o = self.outp("tap_x1_%d" % l, [512, D])
            for r0 in range(0, 512, 128):
                S.dma("sp", o[r0:r0 + 128, :], xdst[r0:r0 + 128, :], reads=["xout"])
        S.barrier()
        ar.release()


def perm_tokens():
    r = np.arange(4)[:, None, None]
    i = np.arange(16)[None, :, None]
    p = np.arange(128)[None, None, :]
    return ((4 * i + r) * 128 + p).reshape(-1)


def col_layout(v):
    return np.ascontiguousarray(v.reshape(-1, 128).T)


def make_in_maps(inputs, layers=(0, 1)):
    perm = perm_tokens()
    inv = (10000.0 ** (-np.arange(0, 64, 2, dtype=np.float32) / 64)).astype(np.float32)
    maps = []
    for c in range(8):
        g, r = c // 4, c % 4
        sh = 2 * r + g
        m = {}
        own = perm[r * 2048:(r + 1) * 2048]
        m["x_own"] = np.ascontiguousarray(inputs["x"][g][own])
        m["c_own"] = col_layout(inputs["c"][g])
        m["pos_perm"] = np.ascontiguousarray(inputs["positions"][g][perm][None, :]).astype(np.int32)
        cols = np.zeros((128, NCOL), np.float32)
        cols[:, COL_INV] = inv[np.arange(128) % 32]
        cols[:, COL_SGN] = np.where((np.arange(128) % 64) < 32, -1.0, 1.0)
        cols[:, COL_NEGPI] = -np.pi
        for l in layers:
            b = COL_L0 + l * COL_PER_L
            cols[:, b + COL_NORMG:b + COL_NORMG + 32] = col_layout(inputs["norm_g"][l])
            cols[:, b + COL_BSHIFT:b + COL_BSHIFT + 32] = col_layout(inputs["b_ada"][l][0:D])
            cols[:, b + COL_BSCALE:b + COL_BSCALE + 32] = col_layout(inputs["b_ada"][l][D:2 * D])
            cols[:, b + COL_BMG:b + COL_BMG + 96] = col_layout(inputs["b_mg"][l])
            cols[:, b + COL_GQ:b + COL_GQ + 6] = col_layout(inputs["mla_gq"][l])
            cols[:, b + COL_GKV:b + COL_GKV + 4] = col_layout(inputs["mla_gkv"][l])
            m["bgate_%d" % l] = np.ascontiguousarray(inputs["b_ada"][l][None, 2 * D:3 * D])
            for name, (K, N, units) in TILED.items():
                R = K // 8
                m["%s_%d" % (name, l)] = np.ascontiguousarray(inputs[name][l][sh * R:(sh + 1) * R])
            hs = [4 * r + i for i in range(4)]
            wuq = inputs["mla_wuq"][l].reshape(768, 16, 192)[:, hs, :]
            wuq = np.concatenate([wuq, wuq[:, :, 160:192], wuq[:, :, 128:160]], axis=2)
            m["wuq_own_%d" % l] = np.ascontiguousarray(wuq.reshape(768, 1024))
            wukv = inputs["mla_wukv"][l].reshape(512, 16, 256)[:, hs, :]
            m["wukv_own_%d" % l] = np.ascontiguousarray(wukv.reshape(512, 1024))
        sidx = np.arange(64)[:, None]
        cidx = np.arange(64)[None, :]
        m["gla_consts"] = np.concatenate([np.where(sidx <= cidx, -1.0 / 16, 0.0), np.where(sidx > cidx, -1.0 / 16, 0.0),
                                          np.where(sidx <= cidx, 1.0, 0.0)], axis=1).astype(np.float32)
        m["final_g_row"] = np.ascontiguousarray(inputs["final_g"][None, :])
        for l in layers:
            m["gla_wg2_%d" % l] = np.ascontiguousarray(np.concatenate(
                [inputs["gla_wg2"][l][:, r * 128:(r + 1) * 128], inputs["gla_bg"][l][None, r * 128:(r + 1) * 128]], axis=0))
            m["gla_gout_%d" % l] = np.ascontiguousarray(inputs["gla_gout"][l][None, :])
        m["cols"] = cols
        m["ident"] = np.eye(128, dtype=np.float32)
        import ml_dtypes
        kp = np.arange(128)[:, None, None, None]
        rkk = np.arange(4)[None, :, None, None]
        rq = np.arange(4)[None, None, :, None]
        qp = np.arange(128)[None, None, None, :]
        mm = ((rkk < rq) | ((rkk == rq) & (kp <= qp))).astype(np.float32)
        m["mla_mask"] = mm.reshape(128, 2048).astype(ml_dtypes.bfloat16)
        qq = np.arange(128)[:, None, None]
        rk3 = np.arange(4)[None, :, None]
        kk = np.arange(128)[None, None, :]
        vis = ((rk3 < r) | ((rk3 == r) & (kk <= qq))).reshape(128, 512)
        m["dsa_dmask"] = np.where(vis, 0.0, -1e30).astype(np.float32)
        m["dsa_c01"] = vis.astype(np.float32).astype(ml_dtypes.bfloat16)
        maps.append(m)
    return maps


_CACHE = {}


def kernel(**inputs):
    inputs = {k: np.asarray(v) for k, v in inputs.items()}
    if "nc" not in _CACHE:
        B = Builder(layers=(0, 1))
        _CACHE["nc"] = B.build()
        _CACHE["B"] = B
    B, nc = _CACHE["B"], _CACHE["nc"]
    maps = make_in_maps(inputs, layers=(0, 1))
    maps = [{k: v for k, v in m.items() if k in B.ins} for m in maps]
    res = run_bass_kernel_spmd(nc, maps, core_ids=list(range(8)))
    perm = perm_tokens()
    out = np.empty((2, SEQ, D), np.float32)
    for c in range(8):
        g, r = c // 4, c % 4
        out[g][perm[r * 2048:(r + 1) * 2048]] = np.asarray(res.results[c]["out_own"], dtype=np.float32)
    return out
```

```python
import numpy as np
from contextlib import ExitStack
import concourse.bass as bass
import concourse.mybir as mybir
from concourse.bass_utils import run_bass_kernel_spmd

F32 = mybir.dt.float32
BF16 = mybir.dt.bfloat16
I32 = mybir.dt.int32
AF = mybir.ActivationFunctionType
ALU = mybir.AluOpType
AX = mybir.AxisListType

D = 4096
SEQ = 8192
NT = 2048
DEPTH = 2
EPS = 1e-6
GROUPS4 = [[0, 1, 2, 3], [4, 5, 6, 7]]
GROUP8 = [list(range(8))]
PAIRS = [[0, 4], [1, 5], [2, 6], [3, 7]]
PI = float(np.pi)


class Sched:
    COMPUTE = ("pe", "act", "dve", "pool")

    def __init__(self, nc, es, n_dma_sems=8):
        self.nc = nc
        self.es = es
        self.epoch = 0
        self.items = {k: [] for k in ("pe", "act", "dve", "pool", "sp")}
        self.sems = {}
        self.cnt = {}
        for k in self.COMPUTE:
            self.sems[k] = es.enter_context(nc.semaphore("s_" + k))
            self.cnt[k] = 0
        self.dq = {}
        for q in ("sp", "pool", "act"):
            ss = [es.enter_context(nc.semaphore("d_%s%d" % (q, i))) for i in range(n_dma_sems)]
            for i, s in enumerate(ss):
                self.sems[("d", q, i)] = s
                self.cnt[("d", q, i)] = 0
            self.dq[q] = 0
        self.sems["cc"] = es.enter_context(nc.semaphore("s_cc"))
        self.cnt["cc"] = 0
        self.nd = n_dma_sems
        self.seen = {k: {} for k in self.items}
        self.st = {}
        self.n_wait = 0
        self.n_ins = 0

    def _need(self, reads, writes, eng=None):
        deps = []
        for k in reads:
            s = self.st.get(k)
            if s and s[0]:
                deps.append(s[0])
        for k in writes:
            s = self.st.get(k)
            if s:
                if s[0] and s[0][0] != eng:
                    deps.append(s[0])
                deps.extend(t for t in s[1] if t[0] != eng)
        return deps

    def _wait(self, eng, deps):
        seen = self.seen[eng]
        best = {}
        for (sk, c) in deps:
            if eng == "pe" and sk == "pe":
                continue
            if seen.get(sk, 0) >= c:
                continue
            if best.get(sk, 0) < c:
                best[sk] = c
        for sk, c in best.items():
            self.items[eng].append(("w", self.sems[sk], c))
            seen[sk] = c
            self.n_wait += 1

    def _record(self, ticket, reads, writes):
        for k in reads:
            s = self.st.setdefault(k, [None, []])
            s[1].append(ticket)
            if len(s[1]) > 64:
                m = {}
                for (sk, c) in s[1]:
                    if m.get(sk, 0) < c:
                        m[sk] = c
                s[1] = list(m.items())
        for k in writes:
            self.st[k] = [ticket, []]

    def op(self, eng, fn, reads=(), writes=()):
        psr = [k for k in reads if isinstance(k, tuple) and k[0] == "ps"]
        if psr:
            writes = list(writes) + psr
        self._wait(eng, self._need(reads, writes, eng))
        self.cnt[eng] += 1
        t = (eng, self.cnt[eng])
        self.items[eng].append(("i", fn, self.sems[eng], 1))
        self._record(t, reads, writes)
        self.n_ins += 1
        return t

    def dma(self, q, out, in_, reads=(), writes=(), **kw):
        n = self.dq[q]
        self.dq[q] += 1
        sk = ("d", q, n % self.nd)
        deps = self._need(reads, writes)
        if self.cnt[sk] > 0:
            deps.append((sk, self.cnt[sk]))
        self._wait(q, deps)
        self.cnt[sk] += 16
        t = (sk, self.cnt[sk])
        self.items[q].append(("i", lambda e: e.dma_start(out=(out(e) if callable(out) else out),
                                                         in_=(in_(e) if callable(in_) else in_), **kw), self.sems[sk], 16))
        self._record(t, reads, writes)
        self.n_ins += 1
        return t

    def collective(self, kind, groups, in_ap, out_ap, reads=(), writes=()):
        self._wait("pool", self._need(reads, writes))
        self.cnt["cc"] += 1
        t = ("cc", self.cnt["cc"])
        self.items["pool"].append(("c", lambda e: e.collective_compute(
            kind, ALU.bypass, replica_groups=groups, ins=[in_ap], outs=[out_ap]), self.sems["cc"], None))
        self._record(t, reads, writes)
        return t

    def barrier(self):
        allt = [(sk, c) for sk, c in self.cnt.items() if c > 0]
        for eng in self.items:
            self._wait(eng, allt)
        self.st = {}
        self.epoch += 1
        for k in self.COMPUTE:
            if self.cnt[k] > 0:
                self.sems[k] = self.es.enter_context(self.nc.semaphore("s_%s_%d" % (k, self.epoch)))
                self.cnt[k] = 0
                for eng in self.seen:
                    self.seen[eng].pop(k, None)

    def wait_all(self, eng):
        self._wait(eng, [(sk, c) for sk, c in self.cnt.items() if c > 0])

    def emit(self, block):
        def run(e, items):
            for it in items:
                if it[0] == "w":
                    e.wait_ge(it[1], it[2])
                elif it[0] == "i":
                    it[1](e).then_inc(it[2], it[3])
                else:
                    it[1](e).then_inc(it[2])

        @block.tensor
        def _(e):
            run(e, self.items["pe"])

        @block.scalar
        def _(e):
            run(e, self.items["act"])

        @block.vector
        def _(e):
            run(e, self.items["dve"])

        @block.gpsimd
        def _(e):
            run(e, self.items["pool"])

        @block.sync
        def _(e):
            run(e, self.items["sp"])


class Arena:
    def __init__(self, tensor_f32, nwords):
        self.t = tensor_f32
        self.n = nwords
        self.off = 0
        self.marks = []

    def alloc(self, nelem, dtype=F32, parts=128):
        words = (nelem + 1) // 2 if dtype == BF16 else nelem
        a = self.off
        self.off += words
        assert self.off <= self.n, "SBUF arena overflow %d > %d" % (self.off, self.n)
        v = self.t[0:parts, a:a + words]
        if dtype != F32:
            v = v.bitcast(dtype)
        return v

    def mark(self):
        self.marks.append(self.off)

    def release(self):
        self.off = self.marks.pop()


class Rot:
    def __init__(self, n):
        self.n = n
        self.i = 0

    def next(self):
        v = self.i % self.n
        self.i += 1
        return v


C_CQ, C_CKV, C_KR, C_GQ, C_GK, C_GV, C_GLOW = 0, 768, 1280, 1344, 1856, 2368, 3392
C_DQ, C_DK, C_DV, C_IQ, C_IK, C_IW, C_Z = 3408, 4432, 4688, 4944, 6992, 7056, 7088
IN_W = 11184

WIN_UNITS = [
    ("cq0", [(0, 512)]), ("cq1", [(512, 768)]), ("ckv", [(768, 1280)]),
    ("kr", [(1280, 1344), (1312, 1344), (1280, 1312), (3392, 3408)]),
    ("gq", [(1344, 1856)]), ("gk", [(1856, 2368)]),
    ("gv0", [(2368, 2880)]), ("gv1", [(2880, 3392)]),
    ("dq0", [(3408, 3920)]), ("dq1", [(3920, 4432)]),
    ("dkik", [(4432, 4688), (6992, 7056)]),
    ("dviw", [(4688, 4944), (7056, 7088)]),
    ("iq0", [(4944, 5456)]), ("iq1", [(5456, 5968)]), ("iq2", [(5968, 6480)]), ("iq3", [(6480, 6992)]),
] + [("z%d" % i, [(C_Z + 512 * i, C_Z + 512 * (i + 1))]) for i in range(8)]


def units_plain(n_cols):
    return [("u%d" % i, [(512 * i, min(n_cols, 512 * (i + 1)))]) for i in range((n_cols + 511) // 512)]


def unit_width(u):
    return sum(b - a for a, b in u[1])


TILED = {
    "w_ada": (4096, 12288, units_plain(12288)),
    "w_in": (4096, IN_W, WIN_UNITS),
    "w_mg": (4096, 12288, units_plain(12288)),
    "w_bm": (2048, 4096, units_plain(4096)),
    "w_bg": (1024, 4096, units_plain(4096)),
    "w_bd": (1024, 4096, units_plain(4096)),
    "w_o": (4096, 4096, units_plain(4096)),
}
PLAIN = {"mla_wuq": (768, 3072), "mla_wukv": (512, 4096)}

COL_INV, COL_SGN, COL_NEGPI = 0, 1, 2
COL_L0 = 4
COL_NORMG, COL_BSHIFT, COL_BSCALE, COL_BMG, COL_GQ, COL_GKV = 0, 32, 64, 96, 192, 198
COL_PER_L = 208
NCOL = COL_L0 + DEPTH * COL_PER_L


def col_of(l, what, i=0):
    return COL_L0 + l * COL_PER_L + what + i


class Builder:
    def __init__(self, layers=(0, 1), taps=(), stop_after=None):
        self.layers = tuple(layers)
        self.taps = set(taps)
        self.stop_after = stop_after
        self.nc = bass.Bass("TRN2", target_bir_lowering=False)
        self.ins = {}
        self.outs = {}
        self.dr = {}

    def inp(self, name, shape, dt=F32):
        t = self.nc.dram_tensor(name, list(shape), dt, kind="ExternalInput")
        self.ins[name] = (tuple(shape), dt)
        return t

    def outp(self, name, shape, dt=F32):
        t = self.nc.dram_tensor(name, list(shape), dt, kind="ExternalOutput")
        self.outs[name] = (tuple(shape), dt)
        return t

    def dram(self, name, shape, dt):
        t = self.nc.dram_tensor(name, list(shape), dt)
        self.dr[name] = t
        return t

    def rank_of(self, e):
        if not hasattr(self, "_rk"):
            self._rk = {}
        k = id(e)
        if k not in self._rk:
            self._rk[k] = e.partition_id() % 4
        return self._rk[k]

    def build(self):
        nc = self.nc
        with ExitStack() as es:
            self.es = es
            AW = 51200
            self.arena_t = es.enter_context(nc.sbuf_tensor("arena", [128, AW], F32))
            self.ar = Arena(self.arena_t, AW)
            self.ps = [es.enter_context(nc.psum_tensor("ps%d" % i, [128, 512], F32)) for i in range(8)]
            self.S = Sched(nc, es)
            self.declare_io()
            self.consts()
            self.phase_W()
            self.phase_R()
            for l in self.layers:
                if self.stop_after == ("W",):
                    break
                self.phase_M(l)
                if self.stop_after == ("M", l):
                    break
                self.phase_A(l)
                if self.stop_after == ("A", l):
                    break
                self.phase_G1(l)
                import os
                if not os.environ.get("SKIP_MLA"):
                    self.phase_MLA(l)
                if self.stop_after == ("MLA", l):
                    break
                self.phase_DSA(l)
                if self.stop_after == ("DSA", l):
                    break
                self.phase_GLA(l)
                if self.stop_after == ("GLA", l):
                    break
                self.phase_G2(l)
                self.phase_C(l, last=(l == DEPTH - 1))
                if self.stop_after == ("C", l):
                    break
            self.S.barrier()
            if self.taps:
                od = self.outp("tap_done", [128, 4])
                self.S.dma("sp", od[:, :], self.cols[:, 0:4], reads=["cols"])
            self.S.wait_all("sp")
            self.S.wait_all("pool")
            with nc.Block() as block:
                self.S.emit(block)
        return nc

    def declare_io(self):
        self.x_own = self.inp("x_own", [NT, D])
        self.c_own = self.inp("c_own", [128, 32])
        self.pos_perm = self.inp("pos_perm", [1, SEQ], I32)
        self.cols_in = self.inp("cols", [128, NCOL])
        self.ident_in = self.inp("ident", [128, 128])
        self.mla_mask_in = self.inp("mla_mask", [128, 2048], BF16)
        self.dsa_dmask_in = self.inp("dsa_dmask", [128, 512])
        self.dsa_c01_in = self.inp("dsa_c01", [128, 512], BF16)
        self.gla_consts_in = self.inp("gla_consts", [64, 192])
        self.final_g_in = self.inp("final_g_row", [1, D])
        self.gla_wg2_in = {}
        self.gla_gout_in = {}
        for l in self.layers:
            self.gla_wg2_in[l] = self.inp("gla_wg2_%d" % l, [17, 128])
            self.gla_gout_in[l] = self.inp("gla_gout_%d" % l, [1, 256])
        self.out_own = self.outp("out_own", [NT, D])
        self.xmid = self.dram("xmid", [NT, D], F32)
        self.rows_in = {}
        for l in self.layers:
            self.rows_in[("bgate", l)] = self.inp("bgate_%d" % l, [1, D])
        self.wsh = {}
        for l in self.layers:
            for name, (K, N, units) in TILED.items():
                self.wsh[(name, l)] = self.inp("%s_%d" % (name, l), [K // 8, N])
            self.wsh[("wuq_own", l)] = self.inp("wuq_own_%d" % l, [768, 1024])
            self.wsh[("wukv_own", l)] = self.inp("wukv_own_%d" % l, [512, 1024])

    def consts(self):
        S, ar = self.S, self.ar
        self.cols = ar.alloc(NCOL)
        S.dma("sp", self.cols, self.cols_in[:, :], writes=["cols"])
        self.ident = ar.alloc(128)
        S.dma("sp", self.ident, self.ident_in[:, :], writes=["ident"])
        self.ones_f = ar.alloc(128)
        S.op("dve", lambda e: e.memset(self.ones_f, 1.0), writes=["ones_f"])
        self.ident_b = ar.alloc(128, BF16)
        S.op("dve", lambda e: e.tensor_copy(out=self.ident_b, in_=self.ident), reads=["ident"], writes=["ident_b"])
        self.ones_b = ar.alloc(128, BF16)
        S.op("dve", lambda e: e.memset(self.ones_b, 1.0), writes=["ones_b"])
        self.a_col = ar.alloc(32)
        self.b_col = ar.alloc(32)

    def gather2(self, name, rows, cols, dt, src_key):
        S = self.S
        wb = self.dr[name + "_b"]
        wm = self.dram(name + "_m", [2 * rows, cols], dt)
        wt = self.dram(name + "_t", [8 * rows, cols], dt)
        S.collective("AllGather", PAIRS, wb.ap().opt(), wm.ap().opt(), reads=[src_key], writes=[("wm", name)])
        S.collective("AllGather", GROUPS4, wm.ap().opt(), wt.ap().opt(), reads=[("wm", name)], writes=[("wt", name)])
        return wt, ("wt", name)

    def phase_W(self):
        S, ar, nc = self.S, self.ar, self.nc
        ar.mark()
        CW = 2048
        nslot = 2
        stg_f = [ar.alloc(4 * CW) for _ in range(nslot)]
        stg_b = [ar.alloc(4 * CW, BF16) for _ in range(nslot)]
        rot = Rot(nslot)
        cast_rot = Rot(2)
        self.WTG = {}
        for l in self.layers:
            for name, (K, N, units) in TILED.items():
                KL = K // 1024
                NU = len(units)
                per_group = 4 // KL
                src = self.wsh[(name, l)]
                srcv = src.ap().rearrange("(k p) n -> p k n", p=128)
                groups = []
                for g0 in range(0, NU, per_group):
                    ng = min(per_group, NU - g0)
                    gname = "w_%s_%d_%d" % (name, l, g0)
                    wb = self.dram(gname + "_b", [ng * 128, KL * 512], BF16)
                    groups.append([gname, g0, ng, wb, None])
                self.WTG[(name, l)] = (groups, per_group, KL)
                pending = {}
                ui = 0
                while ui < NU:
                    grp = []
                    w = 0
                    while ui < NU and w + unit_width(units[ui]) <= CW:
                        grp.append(ui)
                        w += unit_width(units[ui])
                        ui += 1
                    s = rot.next()
                    kf, kb = ("wstg_f", s), ("wstg_b", s)
                    sf = stg_f[s][:, 0:KL * w].rearrange("p (k c) -> p k c", k=KL)
                    sb = stg_b[s][:, 0:KL * w].rearrange("p (k c) -> p k c", k=KL)
                    off = 0
                    offs = {}
                    for u in grp:
                        offs[u] = off
                        for (a, b) in units[u][1]:
                            S.dma("sp", sf[:, :, off:off + (b - a)], srcv[:, :, a:b], writes=[kf])
                            off += b - a
                    ce = ("dve", "act")[cast_rot.next()]
                    if ce == "dve":
                        S.op("dve", lambda e, sb=sb, sf=sf: e.tensor_copy(out=sb, in_=sf), reads=[kf], writes=[kb])
                    else:
                        S.op("act", lambda e, sb=sb, sf=sf: e.activation(out=sb, in_=sf, func=AF.Copy), reads=[kf], writes=[kb])
                    for u in grp:
                        wu = unit_width(units[u])
                        G = groups[u // per_group]
                        j = u % per_group
                        wbv = G[3].ap().rearrange("(j p) f -> j p f", p=128)
                        S.dma("pool", wbv[j, :, 0:KL * wu].rearrange("p (k c) -> p k c", k=KL),
                              sb[:, :, offs[u]:offs[u] + wu], reads=[kb], writes=[("wbu", G[0], j)])
                        pending[G[0]] = pending.get(G[0], 0) + 1
                        if pending[G[0]] == G[2]:
                            for jj in range(G[2]):
                                pass
                            S_keys = [("wbu", G[0], jj) for jj in range(G[2])]
                            wm = self.dram(G[0] + "_m", [2 * G[2] * 128, KL * 512], BF16)
                            wt = self.dram(G[0] + "_t", [8 * G[2] * 128, KL * 512], BF16)
                            S.collective("AllGather", PAIRS, G[3].ap().opt(), wm.ap().opt(), reads=S_keys,
                                         writes=[("wm", G[0])])
                            S.collective("AllGather", GROUPS4, wm.ap().opt(), wt.ap().opt(), reads=[("wm", G[0])],
                                         writes=[("wt", name, l)])
                            G[4] = wt
        if ("W",) in self.taps:
            o = self.outp("tap_w_in", [128, 8 * 4 * 144], BF16)
            S.dma("sp", o[:, :].rearrange("p (r f) -> p r f", r=8), self.wt_unit_ap("w_in", self.layers[0], 3, 144),
                  reads=[("wt", "w_in", self.layers[0])])
            o = self.outp("tap_w_bg", [128, 8 * 512], BF16)
            S.dma("sp", o[:, :].rearrange("p (r f) -> p r f", r=8), self.wt_unit_ap("w_bg", self.layers[0], 6, 512),
                  reads=[("wt", "w_bg", self.layers[0])])
        S.barrier()
        ar.release()

    def wt_unit_ap(self, name, l, u, width):
        groups, per_group, KL = self.WTG[(name, l)]
        G = groups[u // per_group]
        j = u % per_group
        v = G[4].ap().rearrange("(s j p) f -> j p s f", s=8, j=G[2], p=128)
        return v[j, :, :, 0:KL * width]

    def load_unit(self, q, dst, name, l, u, width, writes):
        src = self.wt_unit_ap(name, l, u, width)
        d4 = dst.rearrange("p (r k) c -> p r (k c)", r=8)
        return self.S.dma(q, d4, src, reads=[("wt", name, l)], writes=writes)

    def phase_M(self, l):
        S, ar, ps = self.S, self.ar, self.ps
        ar.mark()
        gate_bc = ar.alloc(D)
        sc = ar.alloc(32)
        scb = ar.alloc(32, BF16)
        screp = ar.alloc(32 * 128, BF16)
        ws = [ar.alloc(32 * 512, BF16) for _ in range(2)]
        modc = ar.alloc(64)
        bg = ar.alloc(D)
        import os
        MCUT = int(os.environ.get("MCUT", "99"))
        if MCUT == -10:
            S.barrier(); ar.release(); return
        S.dma("sp", sc, self.c_own[:, :], writes=["m_sc"])
        if MCUT == -11:
            S.barrier(); ar.release(); return
        S.dma("sp", bg, self.rows_in[("bgate", l)][0:1, :].partition_broadcast(128), writes=["m_bg"])
        if MCUT == -12:
            S.barrier(); ar.release(); return
        S.op("act", lambda e: e.activation(out=sc, in_=sc, func=AF.Silu), reads=["m_sc"], writes=["m_sc"])
        S.op("dve", lambda e: e.tensor_copy(out=scb, in_=sc), reads=["m_sc"], writes=["m_scb"])
        if MCUT == -13:
            S.barrier(); ar.release(); return
        screp3 = screp.rearrange("p (k m) -> p k m", k=32)
        for kc in range(32):
            S.op("dve", lambda e, kc=kc: e.tensor_copy(out=screp3[:, kc, :], in_=scb[:, kc:kc + 1].to_broadcast([128, 128])),
                 reads=["m_scb"], writes=["m_screp"])
        rot = Rot(2)
        for u in range(24):
            if MCUT < 1:
                break
            s = rot.next()
            w3 = ws[s].rearrange("p (k c) -> p k c", k=32)
            self.load_unit("sp", w3, "w_ada", l, u, 512, writes=[("m_ws", s)])
            if MCUT < 2:
                continue
            if u < 16:
                for pc in range(4):
                    j = u * 4 + pc
                    bank = 4 + (j % 2)
                    for kc in range(32):
                        S.op("pe", lambda e, kc=kc, pc=pc, w3=w3, bank=bank: e.matmul(
                            ps[bank][:, 0:1], lhsT=w3[:, kc, pc * 128:(pc + 1) * 128], rhs=scb[:, kc:kc + 1],
                            start=(kc == 0), stop=(kc == 31)),
                            reads=[("m_ws", s), "m_scb"], writes=[("ps", bank)])
                    S.op("dve", lambda e, j=j, bank=bank: e.tensor_copy(out=modc[:, j:j + 1], in_=ps[bank][:, 0:1]),
                         reads=[("ps", bank)], writes=["m_modc"])
            else:
                j = u - 16
                bank = 6 + (j % 2)
                for kc in range(32):
                    S.op("pe", lambda e, kc=kc, w3=w3, bank=bank: e.matmul(
                        ps[bank][:, :], lhsT=screp3[:, kc, :], rhs=w3[:, kc, :],
                        start=(kc == 0), stop=(kc == 31)),
                        reads=[("m_ws", s), "m_screp"], writes=[("ps", bank)])
                S.op("dve", lambda e, j=j, bank=bank: e.tensor_tensor(
                    out=gate_bc[:, j * 512:(j + 1) * 512], in0=ps[bank][:, :], in1=bg[:, j * 512:(j + 1) * 512],
                    op=ALU.add), reads=[("ps", bank), "m_bg"], writes=["gate_bc"])
        if MCUT == -1:
            S.barrier()
            ar.release()
            return
        cN = self.cols[:, col_of(l, COL_NORMG):col_of(l, COL_NORMG) + 32]
        cBsh = self.cols[:, col_of(l, COL_BSHIFT):col_of(l, COL_BSHIFT) + 32]
        cBsc = self.cols[:, col_of(l, COL_BSCALE):col_of(l, COL_BSCALE) + 32]
        S.op("dve", lambda e: e.tensor_tensor(out=self.b_col, in0=modc[:, 0:32], in1=cBsh, op=ALU.add),
             reads=["m_modc", "cols"], writes=["b_col"])
        S.op("dve", lambda e: e.scalar_tensor_tensor(out=self.a_col, in0=modc[:, 32:64], scalar=1.0, in1=cBsc,
                                                     op0=ALU.add, op1=ALU.add),
             reads=["m_modc", "cols"], writes=["a_col"])
        S.op("dve", lambda e: e.tensor_tensor(out=self.a_col, in0=self.a_col, in1=cN, op=ALU.mult),
             reads=["a_col", "cols"], writes=["a_col"])
        if MCUT == -2:
            S.barrier()
            ar.release()
            return
        gdr = self.dram("gatebc_%d" % l, [128, D], F32)
        S.dma("sp", gdr[:, :], gate_bc, reads=["gate_bc"], writes=["gatebc_dram"])
        if MCUT == -3:
            S.barrier()
            ar.release()
            return
        if ("mod", l) in self.taps:
            if MCUT == -6:
                o = self.outp("tap_b_%d" % l, [128, 32])
                S.dma("sp", o[:, :], self.b_col, reads=["b_col"])
            elif MCUT == -7:
                o = self.outp("tap_b_%d" % l, [128, 32])
                bn = self.dram("bounce_tap", [128, 32], F32)
                S.dma("sp", bn[:, :], self.cols[:, 0:32], reads=["cols"], writes=["bnc"])
                S.dma("sp", o[:, :], bn[:, :], reads=["bnc"])
            elif MCUT != -5:
                o = self.outp("tap_mod_%d" % l, [128, 64])
                S.dma("sp", o[:, 0:32], self.a_col, reads=["a_col"])
                S.dma("sp", o[:, 32:64], self.b_col, reads=["b_col"])
            if MCUT not in (-4, -6, -7):
                o2 = self.outp("tap_gate_%d" % l, [128, D])
                S.dma("sp", o2[:, :], gate_bc, reads=["gate_bc"])
        S.barrier()
        ar.release()

    def phase_A(self, l):
        S, ar, ps, nc = self.S, self.ar, self.ps, self.nc
        ar.mark()
        GA_ROWS = 768 + 512 + 512 + 512 + 256 + 64
        self.GA_ROWS = GA_ROWS
        self.GA_OFF = dict(cq=0, ckv=768, gq=1280, gk=1792, dk=2304, ik=2560)
        self.GAF_OFF = dict(kr1=0, kr2=64, glow=128)
        self.GAT_OFF = dict(gk=0, gv=512, dv=1536)
        ga = self.dram("ga_src_%d" % l, [GA_ROWS, NT], BF16)
        gaf = self.dram("gaf_src_%d" % l, [144, NT], F32)
        gat = self.dram("gat_src_%d" % l, [NT, 1792], BF16)
        dqT = self.dram("dqT_%d" % l, [1024, NT], BF16)
        iqT = self.dram("iqT_%d" % l, [2048, NT], BF16)
        zT = self.dram("zT_%d" % l, [4096, NT], BF16)
        gT = self.dram("gT_%d" % l, [12288, NT], BF16)
        iw = self.dram("iw_%d" % l, [NT, 32], F32)
        self.A_out = dict(ga=ga, gaf=gaf, gat=gat, dqT=dqT, iqT=iqT, zT=zT, gT=gT, iw=iw)
        A_meta = dict(ga=([GA_ROWS, NT], BF16), gaf=([144, NT], F32), gat=([NT, 1792], BF16), dqT=([1024, NT], BF16),
                      iqT=([2048, NT], BF16), zT=([4096, NT], BF16), gT=([12288, NT], BF16), iw=([NT, 32], F32))
        xsrc = self.x_own if l == 0 else self.dr["xres%d" % (l - 1)]

        hT = ar.alloc(32 * 1024, BF16)
        hT3 = hT.rearrange("p (k t) -> p k t", k=32)
        ws = [ar.alloc(32 * 512, BF16) for _ in range(2)]
        X = ar.alloc(D)
        junk = ar.alloc(D, BF16)
        ss = ar.alloc(4)
        stg = [ar.alloc(512, BF16) for _ in range(4)]
        stgf = [ar.alloc(512) for _ in range(2)]
        raw = ar.alloc(6 * 512)
        raw3 = raw.rearrange("p (a c) -> p a c", a=6)
        sq = [ar.alloc(512) for _ in range(2)]
        rstd_bc = ar.alloc(512)
        wrot, srot, sfrot, brot, sqrot, evrot = Rot(2), Rot(4), Rot(2), Rot(6), Rot(2), Rot(2)

        def evac_copy(dst, src, reads, writes, force=None):
            if force == "act" or (force is None and evrot.next() == 0):
                S.op("act", lambda e: e.activation(out=dst, in_=src, func=AF.Copy), reads=reads, writes=writes)
            else:
                S.op("dve", lambda e: e.tensor_copy(out=dst, in_=src), reads=reads, writes=writes)

        unit_idx = {u[0]: i for i, u in enumerate(WIN_UNITS)}

        for half in range(2):
            for tt in range(8):
                u0 = half * 1024 + tt * 128
                for hh in range(2):
                    S.dma("sp", X[:, hh * 2048:(hh + 1) * 2048], xsrc[u0:u0 + 128, hh * 2048:(hh + 1) * 2048], writes=["xt"])
                S.op("act", lambda e: e.activation(out=junk, in_=X, func=AF.Square, accum_out=ss[:, 0:1]),
                     reads=["xt"], writes=["junk", "ss0"])
                S.op("act", lambda e: e.activation(out=ss[:, 1:2], in_=ss[:, 0:1], func=AF.Sqrt, scale=1.0 / D, bias=EPS),
                     reads=["ss0"], writes=["ss1"])
                S.op("dve", lambda e: e.reciprocal(out=ss[:, 2:3], in_=ss[:, 1:2]), reads=["ss1"], writes=["ss2"])
                S.op("dve", lambda e: e.tensor_scalar(out=X, in0=X, scalar1=ss[:, 2:3], scalar2=None, op0=ALU.mult),
                     reads=["ss2", "xt"], writes=["xt"])
                for k4 in range(8):
                    bank = k4 % 2 + 6
                    for q in range(4):
                        kc = k4 * 4 + q
                        S.op("pe", lambda e, kc=kc, q=q, bank=bank: e.transpose(
                            out=ps[bank][:, q * 128:(q + 1) * 128], in_=X[:, kc * 128:(kc + 1) * 128],
                            identity=self.ident), reads=["xt", "ident"], writes=[("ps", bank)])
                    for q in range(4):
                        kc = k4 * 4 + q
                        S.op("act", lambda e, kc=kc, q=q, bank=bank, tt=tt: e.activation(
                            out=hT3[:, kc, tt * 128:(tt + 1) * 128], in_=ps[bank][:, q * 128:(q + 1) * 128],
                            func=AF.Identity, scale=self.a_col[:, kc:kc + 1], bias=self.b_col[:, kc:kc + 1]),
                            reads=[("ps", bank), "a_col", "b_col"], writes=[("hT", tt)])
            hreads = [("hT", tt) for tt in range(8)]
            if ("hT", l) in self.taps and half == 0:
                o = self.outp("tap_hT_%d" % l, [128, 32 * 1024], BF16)
                for kc in range(32):
                    S.dma("sp", o[:, kc * 1024:(kc + 1) * 1024], hT3[:, kc, :], reads=hreads)

            def fm_group(w3, wkey, c0, m, tc):
                bank = brot.next()
                for kc in range(32):
                    S.op("pe", lambda e, kc=kc: e.matmul(ps[bank][0:m, :], lhsT=w3[:, kc, c0:c0 + m],
                                                         rhs=hT3[:, kc, tc * 512:(tc + 1) * 512],
                                                         start=(kc == 0), stop=(kc == 31)),
                         reads=[wkey] + hreads, writes=[("ps", bank)])
                return bank

            def tok0(tc):
                return half * 1024 + tc * 512

            def load_win(uname):
                ui = unit_idx[uname]
                width = unit_width(WIN_UNITS[ui])
                s = wrot.next()
                w3 = ws[s][:, 0:32 * width].rearrange("p (k c) -> p k c", k=32)
                self.load_unit("sp", w3, "w_in", l, ui, width, writes=[("ws", s)])
                return w3, ("ws", s)

            def stats_group(pieces, n_feat, gcol, row0):
                for tc in range(2):
                    for pi, (w3, wkey, c0) in enumerate(pieces):
                        bank = fm_group(w3, wkey, c0, 128, tc)
                        evac_copy(raw3[:, pi, :], ps[bank][:, :], [("ps", bank)], [("raw", pi)])
                    bank = brot.next()
                    for pi in range(len(pieces)):
                        q = sqrot.next()
                        S.op("act", lambda e, pi=pi, q=q: e.activation(out=sq[q], in_=raw3[:, pi, :], func=AF.Square),
                             reads=[("raw", pi)], writes=[("sq", q)])
                        S.op("pe", lambda e, q=q, pi=pi, n=len(pieces), bank=bank: e.matmul(
                            ps[bank][:, :], lhsT=self.ones_f, rhs=sq[q], start=(pi == 0), stop=(pi == n - 1)),
                            reads=[("sq", q), "ones_f"], writes=[("ps", bank)])
                    S.op("act", lambda e, bank=bank: e.activation(
                        out=rstd_bc, in_=ps[bank][:, :], func=AF.Sqrt, scale=1.0 / n_feat, bias=EPS),
                        reads=[("ps", bank)], writes=["rstd_bc"])
                    S.op("dve", lambda e: e.reciprocal(out=rstd_bc, in_=rstd_bc), reads=["rstd_bc"], writes=["rstd_bc"])
                    for pi in range(len(pieces)):
                        so = srot.next()
                        S.op("dve", lambda e, pi=pi, so=so: e.scalar_tensor_tensor(
                            out=stg[so], in0=raw3[:, pi, :], scalar=self.cols[:, gcol + pi:gcol + pi + 1],
                            in1=rstd_bc, op0=ALU.mult, op1=ALU.mult),
                            reads=[("raw", pi), "rstd_bc", "cols"], writes=[("stg", so)])
                        S.dma("pool", ga[row0 + pi * 128:row0 + (pi + 1) * 128, tok0(tc):tok0(tc) + 512], stg[so],
                              reads=[("stg", so)], writes=["ga"])

            def fm_pieces(w3, wkey, pieces, func=None):
                for tc in range(2):
                    for (c0, m, dst, dkey, row0) in pieces:
                        bank = fm_group(w3, wkey, c0, m, tc)
                        so = srot.next()
                        if func is not None:
                            S.op("act", lambda e, so=so, bank=bank, m=m: e.activation(
                                out=stg[so][0:m, :], in_=ps[bank][0:m, :], func=func),
                                reads=[("ps", bank)], writes=[("stg", so)])
                        else:
                            evac_copy(stg[so][0:m, :], ps[bank][0:m, :], [("ps", bank)], [("stg", so)])
                        S.dma("pool", dst[row0:row0 + m, tok0(tc):tok0(tc) + 512], stg[so][0:m, :],
                              reads=[("stg", so)], writes=[dkey])

            def tm_unit(w3, wkey, dests, n):
                for tt in range(8):
                    u0 = half * 1024 + tt * 128
                    bank = brot.next()
                    for kc in range(32):
                        S.op("pe", lambda e, kc=kc, bank=bank, tt=tt: e.matmul(
                            ps[bank][:, 0:n], lhsT=hT3[:, kc, tt * 128:(tt + 1) * 128], rhs=w3[:, kc, 0:n],
                            start=(kc == 0), stop=(kc == 31)),
                            reads=[wkey] + hreads, writes=[("ps", bank)])
                    frc = "act" if len(dests) > 1 else None
                    for (c0, w, dst, dkey, dcol, dt) in dests:
                        if dt == BF16:
                            so = srot.next()
                            evac_copy(stg[so][:, 0:w], ps[bank][:, c0:c0 + w], [("ps", bank)], [("stg", so)], force=frc)
                            S.dma("pool", dst[u0:u0 + 128, dcol:dcol + w], stg[so][:, 0:w], reads=[("stg", so)],
                                  writes=[dkey])
                        else:
                            so = sfrot.next()
                            evac_copy(stgf[so][:, 0:w], ps[bank][:, c0:c0 + w], [("ps", bank)], [("stgf", so)], force=frc)
                            S.dma("pool", dst[u0:u0 + 128, dcol:dcol + w], stgf[so][:, 0:w], reads=[("stgf", so)],
                                  writes=[dkey])

            import os
            ACUT = float(os.environ.get("ACUT", "99"))
            if ACUT <= 1:
                break
            wa, ka = load_win("cq0")
            wb_, kb_ = load_win("cq1")
            stats_group([(wa, ka, 0), (wa, ka, 128), (wa, ka, 256), (wa, ka, 384), (wb_, kb_, 0), (wb_, kb_, 128)],
                        768.0, col_of(l, COL_GQ), self.GA_OFF["cq"])
            if ACUT <= 2:
                break
            wa, ka = load_win("ckv")
            stats_group([(wa, ka, 128 * i) for i in range(4)], 512.0, col_of(l, COL_GKV), self.GA_OFF["ckv"])
            wa, ka = load_win("kr")
            for tc in range(2):
                for (c0, m, row0) in ((0, 64, 0), (64, 64, 64), (128, 16, 128)):
                    bank = fm_group(wa, ka, c0, m, tc)
                    so = sfrot.next()
                    evac_copy(stgf[so][0:m, :], ps[bank][0:m, :], [("ps", bank)], [("stgf", so)])
                    S.dma("pool", gaf[row0:row0 + m, tok0(tc):tok0(tc) + 512], stgf[so][0:m, :],
                          reads=[("stgf", so)], writes=["gaf"])
            if ACUT <= 3:
                break
            wa, ka = load_win("gq")
            fm_pieces(wa, ka, [(i * 128, 128, ga, "ga", self.GA_OFF["gq"] + i * 128) for i in range(4)])
            wa, ka = load_win("gk")
            fm_pieces(wa, ka, [(i * 128, 128, ga, "ga", self.GA_OFF["gk"] + i * 128) for i in range(4)])
            tm_unit(wa, ka, [(0, 512, gat, "gat", self.GAT_OFF["gk"], BF16)], 512)
            if ACUT <= 4:
                break
            ASKIP = os.environ.get("ASKIP", "").split(",")
            for j in range(2):
                if "gv" in ASKIP:
                    continue
                wa, ka = load_win("gv%d" % j)
                tm_unit(wa, ka, [(0, 512, gat, "gat", self.GAT_OFF["gv"] + 512 * j, BF16)], 512)
            if ACUT <= 4.5:
                break
            for j in range(2):
                if "dq%d" % j in ASKIP:
                    continue
                wa, ka = load_win("dq%d" % j)
                fm_pieces(wa, ka, [(i * 128, 128, dqT, "dqT", 512 * j + i * 128) for i in range(4)])
            if ACUT <= 5:
                break
            wa, ka = load_win("dkik")
            fm_pieces(wa, ka, [(0, 128, ga, "ga", self.GA_OFF["dk"]), (128, 128, ga, "ga", self.GA_OFF["dk"] + 128),
                               (256, 64, ga, "ga", self.GA_OFF["ik"])])
            wa, ka = load_win("dviw")
            tm_unit(wa, ka, [(0, 256, gat, "gat", self.GAT_OFF["dv"], BF16), (256, 32, iw, "iw", 0, F32)], 288)
            if ACUT <= 6:
                break
            for j in range(4):
                wa, ka = load_win("iq%d" % j)
                fm_pieces(wa, ka, [(i * 128, 128, iqT, "iqT", 512 * j + i * 128) for i in range(4)])
            for j in range(8):
                wa, ka = load_win("z%d" % j)
                fm_pieces(wa, ka, [(i * 128, 128, zT, "zT", 512 * j + i * 128) for i in range(4)], func=AF.Silu)
            if ACUT <= 7:
                break
            for ui in range(24):
                s = wrot.next()
                w3 = ws[s].rearrange("p (k c) -> p k c", k=32)
                wkey = ("ws", s)
                self.load_unit("sp", w3, "w_mg", l, ui, 512, writes=[wkey])
                for tc in range(2):
                    for pc in range(4):
                        j = ui * 4 + pc
                        bank = fm_group(w3, wkey, pc * 128, 128, tc)
                        so = srot.next()
                        bcol = col_of(l, COL_BMG, j)
                        S.op("act", lambda e, so=so, bank=bank, bcol=bcol: e.activation(
                            out=stg[so], in_=ps[bank][:, :], func=AF.Sigmoid, bias=self.cols[:, bcol:bcol + 1]),
                            reads=[("ps", bank), "cols"], writes=[("stg", so)])
                        S.dma("pool", gT[j * 128:(j + 1) * 128, tok0(tc):tok0(tc) + 512], stg[so],
                              reads=[("stg", so)], writes=["gT"])
        for nm in ("ga", "gaf", "gat", "dqT", "iqT", "zT", "gT", "iw"):
            for tp in self.taps:
                if tp[0] == nm and tp[1] == l:
                    t = self.A_out[nm]
                    shape, dt = A_meta[nm]
                    rows = tp[2] if len(tp) > 2 else shape[0]
                    o = self.outp("tap_%s_%d" % (nm, l), [rows, shape[1]], dt)
                    step = 128
                    for r0 in range(0, rows, step):
                        r1 = min(rows, r0 + step)
                        S.dma("sp", o[r0:r1, :], t[r0:r1, :], reads=[nm])
        S.barrier()
        ar.release()


    def phase_R(self):
        S, ar = self.S, self.ar
        ar.mark()
        self.ropeC = self.dram("ropeC", [64, SEQ], F32)
        self.ropeS = self.dram("ropeS", [64, SEQ], F32)
        CH = 2048
        pi_ = ar.alloc(CH)
        pf = ar.alloc(CH)
        t1 = ar.alloc(CH)
        t2 = ar.alloc(CH)
        kf = ar.alloc(CH)
        ki_ = ar.alloc(CH)
        for c in range(SEQ // CH):
            pi32 = pi_.bitcast(I32)
            S.dma("sp", pi32[0:64, :], self.pos_perm[0:1, c * CH:(c + 1) * CH].partition_broadcast(64), writes=["r_pi"])
            S.op("dve", lambda e, pi32=pi32: e.tensor_copy(out=pf[0:64, :], in_=pi32[0:64, :]), reads=["r_pi"], writes=["r_pf"])
            S.op("dve", lambda e: e.tensor_scalar(out=pf[0:64, :], in0=pf[0:64, :], scalar1=self.cols[0:64, COL_INV:COL_INV + 1],
                                                  scalar2=None, op0=ALU.mult), reads=["r_pf", "cols"], writes=["r_pf"])
            ki = ki_.bitcast(I32)
            for (tt_, phi, dst, sgn) in ((t1, 0.5 * PI, self.ropeC, False), (t2, 0.0, self.ropeS, True)):
                key = "r_t1" if tt_ is t1 else "r_t2"
                S.op("dve", lambda e, tt_=tt_, phi=phi: e.tensor_scalar(
                    out=tt_[0:64, :], in0=pf[0:64, :], scalar1=1.0 / (2 * PI), scalar2=phi / (2 * PI) + 0.5,
                    op0=ALU.mult, op1=ALU.add), reads=["r_pf"], writes=[key])
                S.op("dve", lambda e, tt_=tt_: e.tensor_copy(out=ki[0:64, :], in_=tt_[0:64, :]), reads=[key], writes=["r_ki"])
                S.op("dve", lambda e: e.tensor_copy(out=kf[0:64, :], in_=ki[0:64, :]), reads=["r_ki"], writes=["r_kf"])
                S.op("dve", lambda e, tt_=tt_: e.scalar_tensor_tensor(
                    out=tt_[0:64, :], in0=tt_[0:64, :], scalar=-0.5, in1=kf[0:64, :], op0=ALU.add, op1=ALU.subtract),
                    reads=[key, "r_kf"], writes=[key])
                S.op("dve", lambda e, tt_=tt_: e.scalar_tensor_tensor(
                    out=tt_[0:64, :], in0=tt_[0:64, :], scalar=-0.5, in1=tt_[0:64, :], op0=ALU.is_lt, op1=ALU.add),
                    reads=[key], writes=[key])
                S.op("act", lambda e, tt_=tt_: e.activation(out=tt_[0:64, :], in_=tt_[0:64, :], func=AF.Sin, scale=2 * PI),
                     reads=[key], writes=[key])
                if sgn:
                    S.op("dve", lambda e, tt_=tt_: e.tensor_scalar(
                        out=tt_[0:64, :], in0=tt_[0:64, :], scalar1=self.cols[0:64, COL_SGN:COL_SGN + 1], scalar2=None,
                        op0=ALU.mult), reads=[key, "cols"], writes=[key])
                S.dma("pool", dst[:, c * CH:(c + 1) * CH], tt_[0:64, :], reads=[key],
                      writes=["ropeC" if dst is self.ropeC else "ropeS"])
        if ("rope",) in self.taps:
            o = self.outp("tap_ropeC", [64, SEQ])
            S.dma("sp", o[:, :], self.ropeC[:, :], reads=["ropeC"])
            o = self.outp("tap_ropeS", [64, SEQ])
            S.dma("sp", o[:, :], self.ropeS[:, :], reads=["ropeS"])
        S.barrier()
        ar.release()

    def gather_rows(self, name, src, src_key, rows, cols, dt, chunk_rows):
        S = self.S
        out = []
        for r0 in range(0, rows, chunk_rows):
            cr = min(chunk_rows, rows - r0)
            g = self.dram("%s_g%d" % (name, r0), [4 * cr, cols], dt)
            S.collective("AllGather", GROUPS4, src[r0:r0 + cr, :], g.ap().opt(), reads=[src_key], writes=[(name, "g")])
            out.append((g, r0, cr))
        return out

    def phase_G1(self, l):
        A = self.A_out
        self.GAg = self.gather_rows("ga%d" % l, A["ga"], "ga", self.GA_ROWS, NT, BF16, 256)
        self.GAfg = self.gather_rows("gaf%d" % l, A["gaf"], "gaf", 144, NT, F32, 128)
        self.GAtg = self.gather_rows("gat%d" % l, A["gat"], "gat", NT, 1792, BF16, 256)
        self.gb = self.dram("gb_src_%d" % l, [768, SEQ], BF16)

    def ga_ap(self, row0, nrows, rk, u0, n):
        j = row0 // 256
        g, r0, cr = self.GAg[j]
        o = row0 - r0
        return g[rk * cr + o:rk * cr + o + nrows, u0:u0 + n]

    def gaf_ap(self, row0, nrows, rk, u0, n):
        j = row0 // 128
        g, r0, cr = self.GAfg[j]
        o = row0 - r0
        return g[rk * cr + o:rk * cr + o + nrows, u0:u0 + n]

    def phase_MLA(self, l):
        S, ar, ps = self.S, self.ar, self.ps
        ar.mark()
        gb = self.gb
        wq_f = ar.alloc(6 * 1024)
        wq = ar.alloc(6 * 1024, BF16)
        wkv = ar.alloc(4 * 1024, BF16)
        wq3 = wq.rearrange("p (k c) -> p k c", k=6)
        wkv3 = wkv.rearrange("p (k c) -> p k c", k=4)
        wqf3 = wq_f.rearrange("p (k c) -> p k c", k=6)
        S.dma("sp", wqf3, self.wsh[("wuq_own", l)].ap().rearrange("(k p) n -> p k n", p=128), writes=["wq_f"])
        S.op("dve", lambda e: e.tensor_copy(out=wq, in_=wq_f), reads=["wq_f"], writes=["wq"])
        wkvf3 = wq_f[:, 0:4096].rearrange("p (k c) -> p k c", k=4)
        S.dma("sp", wkvf3, self.wsh[("wukv_own", l)].ap().rearrange("(k p) n -> p k n", p=128), reads=["wq"], writes=["wq_f"])
        S.op("dve", lambda e: e.tensor_copy(out=wkv, in_=wq_f[:, 0:4096]), reads=["wq_f"], writes=["wkv"])
        masks = ar.alloc(4 * 512, BF16)
        S.dma("sp", masks, self.mla_mask_in[:, :], writes=["masks"])
        masks3 = masks.rearrange("p (r c) -> p r c", r=4)

        QTn = ar.alloc(SEQ, BF16)
        QTr = ar.alloc(SEQ, BF16)
        KTn = ar.alloc(SEQ, BF16)
        KTr = ar.alloc(SEQ, BF16)
        V = ar.alloc(64 * 128, BF16)
        V3 = V.rearrange("p (t d) -> p t d", t=64)
        cqc = [ar.alloc(6 * 512, BF16) for _ in range(2)]
        ckc = [ar.alloc(4 * 512, BF16) for _ in range(2)]
        krc = [ar.alloc(2 * 512) for _ in range(2)]
        rc = [ar.alloc(2 * 512) for _ in range(2)]
        tmpa = ar.alloc(512)
        tmpb = ar.alloc(512)
        PT = [ar.alloc(512, BF16) for _ in range(5)]
        rec = ar.alloc(512)
        ost = [ar.alloc(512, BF16) for _ in range(2)]
        SCALE = 192.0 ** -0.5
        inrot, ptrot, orot = Rot(2), Rot(5), Rot(2)

        for hh in range(4):
            for tch in range(16):
                rk, u0 = tch // 4, 512 * (tch % 4)
                tcol = tch * 512
                si = inrot.next()
                cq3 = cqc[si].rearrange("p (k c) -> p k c", k=6)
                ck3 = ckc[si].rearrange("p (k c) -> p k c", k=4)
                for kc in range(6):
                    S.dma("sp", cq3[:, kc, :], self.ga_ap(self.GA_OFF["cq"] + kc * 128, 128, rk, u0, 512),
                          reads=[("ga%d" % l, "g")], writes=[("cqc", si)])
                for kc in range(4):
                    S.dma("sp", ck3[:, kc, :], self.ga_ap(self.GA_OFF["ckv"] + kc * 128, 128, rk, u0, 512),
                          reads=[("ga%d" % l, "g")], writes=[("ckc", si)])
                S.dma("sp", rc[si][0:64, 0:512], self.ropeC[:, tcol:tcol + 512], reads=["ropeC"], writes=[("rc", si)])
                S.dma("sp", rc[si][0:64, 512:1024], self.ropeS[:, tcol:tcol + 512], reads=["ropeS"], writes=[("rc", si)])
                if hh == 0:
                    S.dma("sp", krc[si][0:64, 0:512], self.gaf_ap(0, 64, rk, u0, 512), reads=[("gaf%d" % l, "g")],
                          writes=[("krc", si)])
                    S.dma("sp", krc[si][0:64, 512:1024], self.gaf_ap(64, 64, rk, u0, 512), reads=[("gaf%d" % l, "g")],
                          writes=[("krc", si)])
                    S.op("dve", lambda e, si=si: e.tensor_tensor(out=tmpa[0:64, :], in0=krc[si][0:64, 0:512],
                                                                 in1=rc[si][0:64, 0:512], op=ALU.mult),
                         reads=[("krc", si), ("rc", si)], writes=["tmpa"])
                    S.op("dve", lambda e, si=si: e.tensor_tensor(out=tmpb[0:64, :], in0=krc[si][0:64, 512:1024],
                                                                 in1=rc[si][0:64, 512:1024], op=ALU.mult),
                         reads=[("krc", si), ("rc", si)], writes=["tmpb"])
                    S.op("dve", lambda e, tcol=tcol: e.tensor_tensor(out=KTr[0:64, tcol:tcol + 512], in0=tmpa[0:64, :],
                                                                     in1=tmpb[0:64, :], op=ALU.add),
                         reads=["tmpa", "tmpb"], writes=[("KTr", tch)])
                cb = hh * 256
                for kc in range(6):
                    S.op("pe", lambda e, kc=kc, cq3=cq3, cb=cb: e.matmul(ps[0][:, :], lhsT=wq3[:, kc, cb:cb + 128], rhs=cq3[:, kc, :],
                                                                       start=(kc == 0), stop=(kc == 5)),
                         reads=["wq", ("cqc", si)], writes=[("ps", 0)])
                S.op("act", lambda e, tcol=tcol: e.activation(out=QTn[:, tcol:tcol + 512], in_=ps[0][:, :], func=AF.Copy),
                     reads=[("ps", 0)], writes=[("QTn", tch)])
                for (bank, c0) in ((1, cb + 128), (2, cb + 192)):
                    for kc in range(6):
                        S.op("pe", lambda e, kc=kc, cq3=cq3, bank=bank, c0=c0: e.matmul(
                            ps[bank][0:64, :], lhsT=wq3[:, kc, c0:c0 + 64], rhs=cq3[:, kc, :], start=(kc == 0), stop=(kc == 5)),
                            reads=["wq", ("cqc", si)], writes=[("ps", bank)])
                S.op("dve", lambda e, si=si: e.tensor_tensor(out=tmpa[0:64, :], in0=ps[1][0:64, :], in1=rc[si][0:64, 0:512],
                                                             op=ALU.mult), reads=[("ps", 1), ("rc", si)], writes=["tmpa"])
                S.op("dve", lambda e, si=si: e.tensor_tensor(out=tmpb[0:64, :], in0=ps[2][0:64, :], in1=rc[si][0:64, 512:1024],
                                                             op=ALU.mult), reads=[("ps", 2), ("rc", si)], writes=["tmpb"])
                S.op("dve", lambda e, tcol=tcol: e.tensor_tensor(out=QTr[0:64, tcol:tcol + 512], in0=tmpa[0:64, :],
                                                                 in1=tmpb[0:64, :], op=ALU.add),
                     reads=["tmpa", "tmpb"], writes=[("QTr", tch)])
                kb = hh * 256
                for kc in range(4):
                    S.op("pe", lambda e, kc=kc, ck3=ck3, kb=kb: e.matmul(ps[3][:, :], lhsT=wkv3[:, kc, kb:kb + 128], rhs=ck3[:, kc, :],
                                                                       start=(kc == 0), stop=(kc == 3)),
                         reads=["wkv", ("ckc", si)], writes=[("ps", 3)])
                S.op("act", lambda e, tcol=tcol: e.activation(out=KTn[:, tcol:tcol + 512], in_=ps[3][:, :], func=AF.Copy),
                     reads=[("ps", 3)], writes=[("KTn", tch)])
                for ts_ in range(4):
                    for kc in range(4):
                        S.op("pe", lambda e, kc=kc, ck3=ck3, kb=kb, ts_=ts_: e.matmul(
                            ps[4][:, ts_ * 128:(ts_ + 1) * 128], lhsT=ck3[:, kc, ts_ * 128:(ts_ + 1) * 128],
                            rhs=wkv3[:, kc, kb + 128:kb + 256], start=(kc == 0), stop=(kc == 3)),
                            reads=["wkv", ("ckc", si)], writes=[("ps", 4)])
                S.op("act", lambda e, tch=tch: e.activation(out=V[:, tch * 512:(tch + 1) * 512], in_=ps[4][:, :], func=AF.Copy),
                     reads=[("ps", 4)], writes=[("V", tch)])
            allk = lambda nm: [(nm, t) for t in range(16)]
            QTn4 = QTn.rearrange("p (r u) -> p r u", r=4)
            QTr4 = QTr.rearrange("p (r u) -> p r u", r=4)
            for I in range(16):
                qn = QTn4[:, :, I * 128:(I + 1) * 128]
                qr = QTr4[0:64, :, I * 128:(I + 1) * 128]
                tiles = [(rk_, ik_, None) for ik_ in range(I) for rk_ in range(4)] + [(rk_, I, rk_) for rk_ in range(4)]
                nt_ = len(tiles)
                def issue_S(ti):
                    rk_, ik_, mk = tiles[ti]
                    kcol = rk_ * 2048 + ik_ * 128
                    sb_ = (3, 4, 5, 6)[ti % 4]
                    S.op("pe", lambda e, kcol=kcol, sb_=sb_, qn=qn: e.matmul(ps[sb_][:, :], lhsT=KTn[:, kcol:kcol + 128], rhs=qn,
                                                                           start=True, stop=False),
                         reads=allk("KTn") + allk("QTn"), writes=[("ps", sb_)])
                    S.op("pe", lambda e, kcol=kcol, sb_=sb_, qr=qr: e.matmul(ps[sb_][:, :], lhsT=KTr[0:64, kcol:kcol + 128], rhs=qr,
                                                                           start=False, stop=True),
                         reads=allk("KTr") + allk("QTr"), writes=[("ps", sb_)])

                def issue_rest(ti):
                    rk_, ik_, mk = tiles[ti]
                    kcol = rk_ * 2048 + ik_ * 128
                    vt = kcol // 128
                    sb_ = (3, 4, 5, 6)[ti % 4]
                    pi = ptrot.next()
                    S.op("act", lambda e, pi=pi, sb_=sb_: e.activation(out=PT[pi], in_=ps[sb_][:, :], func=AF.Exp, scale=SCALE),
                         reads=[("ps", sb_)], writes=[("PT", pi)])
                    if mk is not None:
                        S.op("pool", lambda e, pi=pi, mk=mk: e.tensor_tensor(out=PT[pi], in0=PT[pi], in1=masks3[:, mk, :],
                                                                             op=ALU.mult),
                             reads=[("PT", pi), "masks"], writes=[("PT", pi)])
                    S.op("pe", lambda e, pi=pi, vt=vt, ti=ti, nt_=nt_: e.matmul(ps[0][:, :], lhsT=V3[:, vt, :], rhs=PT[pi],
                                                                               start=(ti == 0), stop=(ti == nt_ - 1)),
                         reads=[("PT", pi)] + allk("V"), writes=[("ps", 0)])
                    S.op("pe", lambda e, pi=pi, ti=ti, nt_=nt_: e.matmul(ps[1][:, :], lhsT=self.ones_b, rhs=PT[pi],
                                                                        start=(ti == 0), stop=(ti == nt_ - 1)),
                         reads=[("PT", pi), "ones_b"], writes=[("ps", 1)])

                for t0_ in range(min(4, nt_)):
                    issue_S(t0_)
                for ti in range(nt_):
                    issue_rest(ti)
                    if ti + 4 < nt_:
                        issue_S(ti + 4)
                S.op("dve", lambda e: e.reciprocal(out=rec, in_=ps[1][:, :]), reads=[("ps", 1)], writes=["rec"])
                oi = orot.next()
                S.op("dve", lambda e, oi=oi: e.tensor_tensor(out=ost[oi], in0=ps[0][:, :], in1=rec, op=ALU.mult),
                     reads=[("ps", 0), "rec"], writes=[("ost", oi)])
                gbv = gb.ap().rearrange("c (r u) -> c r u", r=4)
                S.dma("pool", gbv[hh * 128:(hh + 1) * 128, :, I * 128:(I + 1) * 128],
                      ost[oi].rearrange("p (r q) -> p r q", r=4), reads=[("ost", oi)], writes=["gb"])
        if ("gb", l) in self.taps:
            o = self.outp("tap_gb_%d" % l, [512, SEQ], BF16)
            for r0 in range(0, 512, 128):
                S.dma("sp", o[r0:r0 + 128, :], gb[r0:r0 + 128, :], reads=["gb"])
        S.barrier()
        ar.release()


    def phase_DSA(self, l):
        S, ar, ps = self.S, self.ar, self.ps
        ar.mark()
        odT = self.dram("odsaT_%d" % l, [1024, NT], BF16)
        self.odT = odT
        A = self.A_out
        gkey = ("ga%d" % l, "g")
        dk = ar.alloc(2 * SEQ, BF16)
        dk3 = dk.rearrange("p (g t) -> p g t", g=2)
        ik = ar.alloc(SEQ, BF16)
        ik4 = ik.rearrange("p (r u) -> p r u", r=4)
        dvt = ar.alloc(64 * 256, BF16)
        dvt3 = dvt.rearrange("p (t c) -> p t c", t=64)
        for g_ in range(2):
            for rk in range(4):
                S.dma("sp", dk3[:, g_, rk * 2048:(rk + 1) * 2048], self.ga_ap(self.GA_OFF["dk"] + g_ * 128, 128, rk, 0, 2048),
                      reads=[gkey], writes=["dk"])
        for rk in range(4):
            S.dma("sp", ik[0:64, rk * 2048:(rk + 1) * 2048], self.ga_ap(self.GA_OFF["ik"], 64, rk, 0, 2048), reads=[gkey],
                  writes=["ik"])
        for rk in range(4):
            for i in range(16):
                g, r0, cr = self.GAtg[i // 2]
                o = (i % 2) * 128
                S.dma("sp", dvt3[:, rk * 16 + i, :], g[rk * cr + o:rk * cr + o + 128, 1536:1792],
                      reads=[("gat%d" % l, "g")], writes=["dvt"])
        dmask = ar.alloc(512)
        S.dma("sp", dmask, self.dsa_dmask_in[:, :], writes=["dmask"])
        c01 = ar.alloc(512, BF16)
        S.dma("sp", c01, self.dsa_c01_in[:, :], writes=["c01"])
        iq_s = ar.alloc(32 * 128, BF16)
        iq3 = iq_s.rearrange("p (h q) -> p h q", h=32)
        iw_s = ar.alloc(32)
        dq_s = ar.alloc(8 * 128, BF16)
        dq3 = dq_s.rearrange("p (h q) -> p h q", h=8)
        Dg = ar.alloc(32 * 128, BF16)
        Dg3 = Dg.rearrange("p (h q) -> p h q", h=32)
        w = ar.alloc(SEQ)
        m8 = ar.alloc(8)
        maskq = ar.alloc(SEQ, BF16)
        maskT = ar.alloc(16 * 512, BF16)
        maskT3 = maskT.rearrange("p (k c) -> p k c", k=16)
        Ah = [ar.alloc(512, BF16) for _ in range(4)]
        PT = [ar.alloc(512, BF16) for _ in range(3)]
        rec = ar.alloc(512)
        ost = [ar.alloc(512, BF16) for _ in range(2)]
        arot, ptrot, orot = Rot(4), Rot(3), Rot(2)
        SCALE = 128.0 ** -0.5
        IMM = -2.0e30
        ps7b = ps[7][:, :].bitcast(BF16)
        iqv = A["iqT"].ap().rearrange("(h d) u -> d h u", d=64)
        dqv = A["dqT"].ap().rearrange("(h d) u -> d h u", d=128)
        odv = odT.ap().rearrange("(h d) u -> d h u", d=128)
        dq_s2 = ar.alloc(8 * 128, BF16)
        dq3l = [dq3, dq_s2.rearrange("p (h q) -> p h q", h=8)]

        def st_load_idx(i):
                L = (i + 1) * 512
                q0 = i * 128
                S.dma("sp", iq3[0:64, :, :], iqv[:, :, q0:q0 + 128], reads=["iqT"], writes=["iq_s"])
                S.dma("sp", iw_s, A["iw"][q0:q0 + 128, :], reads=["iw"], writes=["iw_s"])
                S.dma("sp", dq3l[i % 2], dqv[:, :, q0:q0 + 128], reads=["dqT"], writes=[("dq_s", i % 2)])
                for h in range(32):
                    S.op("dve", lambda e, h=h: e.tensor_scalar(out=Dg3[:, h, :], in0=self.ident_b, scalar1=iw_s[:, h:h + 1],
                                                              scalar2=None, op0=ALU.mult),
                         reads=["ident_b", "iw_s"], writes=["Dg"])
                for kg in range(i + 1):
                    kc_ = ik4[0:64, :, kg * 128:(kg + 1) * 128]
                    accb = 4

                    def idx_S(h, kc_=kc_):
                        sb_ = (2, 3, 5, 6)[h % 4]
                        S.op("pe", lambda e, h=h, sb_=sb_, kc_=kc_: e.matmul(ps[sb_][:, :], lhsT=iq3[0:64, h, :], rhs=kc_,
                                                                           start=True, stop=True),
                             reads=["iq_s", "ik"], writes=[("ps", sb_)])

                    for h0 in range(4):
                        idx_S(h0)
                    for h in range(32):
                        sb_ = (2, 3, 5, 6)[h % 4]
                        ai = arot.next()
                        if h % 2 == 0:
                            S.op("act", lambda e, ai=ai, sb_=sb_: e.activation(out=Ah[ai], in_=ps[sb_][:, :], func=AF.Relu),
                                 reads=[("ps", sb_)], writes=[("Ah", ai)])
                        else:
                            S.op("dve", lambda e, ai=ai, sb_=sb_: e.tensor_scalar(out=Ah[ai], in0=ps[sb_][:, :], scalar1=0.0,
                                                                               scalar2=None, op0=ALU.max),
                                 reads=[("ps", sb_)], writes=[("Ah", ai)])
                        S.op("pe", lambda e, h=h, ai=ai, accb=accb: e.matmul(ps[accb][:, :], lhsT=Dg3[:, h, :], rhs=Ah[ai],
                                                                            start=(h == 0), stop=(h == 31)),
                             reads=["Dg", ("Ah", ai)], writes=[("ps", accb)])
                        if h + 4 < 32:
                            idx_S(h + 4)
                    if kg < i:
                        S.op("act", lambda e, kg=kg, accb=accb: e.activation(out=w[:, kg * 512:(kg + 1) * 512], in_=ps[accb][:, :], func=AF.Copy),
                             reads=[("ps", accb)], writes=["w"])
                    else:
                        S.op("dve", lambda e, kg=kg, accb=accb: e.tensor_tensor(out=w[:, kg * 512:(kg + 1) * 512], in0=ps[accb][:, :], in1=dmask,
                                                                               op=ALU.add), reads=[("ps", accb), "dmask"], writes=["w"])

        def st_topk(i):
                L = (i + 1) * 512
                for rd in range(32):
                    S.op("dve", lambda e, L=L: e.max(out=m8, in_=w[:, 0:L]), reads=["w"], writes=["m8"])
                    S.op("dve", lambda e, L=L: e.match_replace(out=w[:, 0:L], in_to_replace=m8, in_values=w[:, 0:L], imm_value=IMM),
                         reads=["w", "m8"], writes=["w"])
                S.op("dve", lambda e, L=L: e.tensor_scalar(out=maskq[:, 0:L], in0=w[:, 0:L], scalar1=-1.5e30, scalar2=None,
                                                          op0=ALU.is_lt), reads=["w"], writes=["maskq"])
                S.op("dve", lambda e, i=i: e.tensor_tensor(out=maskq[:, i * 512:(i + 1) * 512], in0=maskq[:, i * 512:(i + 1) * 512],
                                                          in1=c01, op=ALU.mult), reads=["maskq", "c01"], writes=["maskq"])

        def st_T(i):
                for kg in range(i + 1):
                    for rk in range(4):
                        S.op("pe", lambda e, kg=kg, rk=rk: e.transpose(out=ps7b[:, rk * 128:(rk + 1) * 128],
                                                                      in_=maskq[:, kg * 512 + rk * 128:kg * 512 + (rk + 1) * 128],
                                                                      identity=self.ident_b),
                             reads=["maskq", "ident_b"], writes=[("ps", 7)])
                    S.op("act", lambda e, kg=kg: e.activation(out=maskT3[:, kg, :], in_=ps7b[:, 0:512], func=AF.Copy),
                         reads=[("ps", 7)], writes=[("maskT", kg)])

        def st_att(i):
                q0 = i * 128
                for g_ in range(2):
                    nt_ = 4 * (i + 1)
                    tl = [(kg, rk) for kg in range(i + 1) for rk in range(4)]

                    def att_S(ti, g_=g_):
                        kg, rk = tl[ti]
                        col = rk * 2048 + kg * 128
                        sb_ = 5 + (ti % 2)
                        S.op("pe", lambda e, col=col, sb_=sb_, g_=g_: e.matmul(ps[sb_][:, :], lhsT=dk3[:, g_, col:col + 128],
                                                                             rhs=dq3l[i % 2][:, 4 * g_:4 * g_ + 4, :], start=True, stop=True),
                             reads=["dk", ("dq_s", i % 2)], writes=[("ps", sb_)])

                    def att_rest(ti, g_=g_, nt_=nt_):
                        kg, rk = tl[ti]
                        tile_ = rk * 16 + kg
                        sb_ = 5 + (ti % 2)
                        pi = ptrot.next()
                        S.op("act", lambda e, pi=pi, sb_=sb_: e.activation(out=PT[pi], in_=ps[sb_][:, :], func=AF.Exp, scale=SCALE),
                             reads=[("ps", sb_)], writes=[("PT", pi)])
                        for hq in range(4):
                            S.op("pool", lambda e, pi=pi, kg=kg, rk=rk, hq=hq: e.tensor_tensor(
                                out=PT[pi][:, hq * 128:(hq + 1) * 128], in0=PT[pi][:, hq * 128:(hq + 1) * 128],
                                in1=maskT3[:, kg, rk * 128:(rk + 1) * 128], op=ALU.mult),
                                reads=[("PT", pi), ("maskT", kg)], writes=[("PT", pi)])
                        S.op("pe", lambda e, pi=pi, tile_=tile_, ti=ti, nt_=nt_, g_=g_: e.matmul(
                            ps[2 * g_][:, :], lhsT=dvt3[:, tile_, g_ * 128:(g_ + 1) * 128], rhs=PT[pi], start=(ti == 0), stop=(ti == nt_ - 1)),
                            reads=[("PT", pi), "dvt"], writes=[("ps", 2 * g_)])
                        S.op("pe", lambda e, pi=pi, ti=ti, nt_=nt_, g_=g_: e.matmul(ps[2 * g_ + 1][:, :], lhsT=self.ones_b, rhs=PT[pi],
                                                                            start=(ti == 0), stop=(ti == nt_ - 1)),
                             reads=[("PT", pi), "ones_b"], writes=[("ps", 2 * g_ + 1)])

                    att_S(0)
                    att_S(1)
                    for ti in range(nt_):
                        att_rest(ti)
                        if ti + 2 < nt_:
                            att_S(ti + 2)
                    S.op("dve", lambda e, g_=g_: e.reciprocal(out=rec, in_=ps[2 * g_ + 1][:, :]), reads=[("ps", 2 * g_ + 1)], writes=["rec"])
                    oi = orot.next()
                    S.op("dve", lambda e, oi=oi, g_=g_: e.tensor_tensor(out=ost[oi], in0=ps[2 * g_][:, :], in1=rec, op=ALU.mult),
                         reads=[("ps", 2 * g_), "rec"], writes=[("ost", oi)])
                    S.dma("pool", odv[:, 4 * g_:4 * g_ + 4, q0:q0 + 128], ost[oi].rearrange("p (h q) -> p h q", h=4),
                          reads=[("ost", oi)], writes=["odT"])

        for i in range(16):
            st_load_idx(i)
            st_topk(i)
            if i > 0:
                st_att(i - 1)
            st_T(i)
        st_att(15)
        if ("odT", l) in self.taps:
            o = self.outp("tap_odT_%d" % l, [1024, NT], BF16)
            for r0 in range(0, 1024, 128):
                S.dma("sp", o[r0:r0 + 128, :], odT[r0:r0 + 128, :], reads=["odT"])
        S.barrier()
        ar.release()


    def phase_GLA(self, l):
        S, ar, ps = self.S, self.ar, self.ps
        ar.mark()
        gkey = ("ga%d" % l, "g")
        gb = self.gb
        qk_all = self.dram("glaqk_all_%d" % l, [4 * 256, SEQ], BF16)
        qk_own = self.dram("glaqk_own_%d" % l, [256, SEQ], BF16)
        tok_own = self.dram("glatok_own_%d" % l, [SEQ, 384], BF16)
        for hd in range(4):
            for wi, nm in enumerate(("gq", "gk")):
                for rk in range(4):
                    S.dma("sp", qk_all[hd * 256 + wi * 128:hd * 256 + (wi + 1) * 128, rk * 2048:(rk + 1) * 2048],
                          self.ga_ap(self.GA_OFF[nm] + hd * 128, 128, rk, 0, 2048), reads=[gkey], writes=["qk_all"])

        def hd_of(e):
            return self.rank_of(e)

        for c4 in range(4):
            S.dma("pool", qk_own[:, c4 * 2048:(c4 + 1) * 2048],
                  lambda e, c4=c4: qk_all[bass.ds(hd_of(e) * 256, 256), c4 * 2048:(c4 + 1) * 2048],
                  reads=["qk_all"], writes=["qk_own"])
        for rk in range(4):
            for j in range(8):
                g, r0, cr = self.GAtg[j]
                rows = slice(rk * cr, (rk + 1) * cr)
                t0 = rk * 2048 + j * 256
                S.dma("pool", tok_own[t0:t0 + 256, 0:128],
                      lambda e, g=g, rows=rows: g[rows, bass.ds(hd_of(e) * 128, 128)],
                      reads=[("gat%d" % l, "g")], writes=["tok_own"])
                S.dma("pool", tok_own[t0:t0 + 256, 128:384],
                      lambda e, g=g, rows=rows: g[rows, bass.ds(512 + hd_of(e) * 256, 256)],
                      reads=[("gat%d" % l, "g")], writes=["tok_own"])
        U = ar.alloc(64)
        Lm = ar.alloc(64)
        tri = ar.alloc(64)
        S.dma("sp", U[0:64, :], self.gla_consts_in[0:64, 0:64], writes=["U"])
        S.dma("sp", Lm[0:64, :], self.gla_consts_in[0:64, 64:128], writes=["Lm"])
        S.dma("sp", tri[0:64, :], self.gla_consts_in[0:64, 128:192], writes=["tri"])
        wg2 = ar.alloc(128)
        bg = ar.alloc(128)
        S.dma("sp", wg2[0:16, :], self.gla_wg2_in[l][0:16, :], writes=["wg2"])
        S.dma("sp", bg[0:1, :], self.gla_wg2_in[l][16:17, :], writes=["bgr"])
        gout = ar.alloc(256)
        S.dma("sp", gout[0:64, :], self.gla_gout_in[l][0:1, :].partition_broadcast(64), writes=["gout"])
        state = ar.alloc(256)
        state_b = ar.alloc(256, BF16)
        S.op("dve", lambda e: e.memset(state, 0.0), writes=["state"])
        S.op("dve", lambda e: e.memset(state_b, 0.0), writes=["state_b"])
        NS = 2
        glw = [ar.alloc(64) for _ in range(NS)]
        qT = [ar.alloc(64, BF16) for _ in range(NS)]
        kT = [ar.alloc(64, BF16) for _ in range(NS)]
        tk = [ar.alloc(384, BF16) for _ in range(NS)]
        sp_ = ar.alloc(128)
        E1 = ar.alloc(64)
        E2 = ar.alloc(64)
        E3 = ar.alloc(128)
        qd = ar.alloc(64, BF16)
        kinv = ar.alloc(64, BF16)
        kend = ar.alloc(128, BF16)
        attT = ar.alloc(64, BF16)
        junk = ar.alloc(256)
        ssq = ar.alloc(4)
        on = ar.alloc(256, BF16)
        ostg = [ar.alloc(128, BF16) for _ in range(2)]
        ps7b = ps[7][:, :].bitcast(BF16)
        rot, orot = Rot(NS), Rot(2)
        DKS = 128.0 ** -0.5
        gfkey = ("gaf%d" % l, "g")
        gfg, gfr0, gfcr = self.GAfg[1]
        for n in range(128):
            j = n // 2
            rk, i = j % 4, j // 4
            u0 = i * 128 + (n % 2) * 64
            tau0 = rk * 2048 + u0
            s_ = rot.next()
            S.dma("sp", glw[s_][0:16, :], gfg[rk * gfcr:rk * gfcr + 16, u0:u0 + 64], reads=[gfkey], writes=[("glw", s_)])
            S.dma("sp", qT[s_], qk_own[0:128, tau0:tau0 + 64], reads=["qk_own"], writes=[("qT", s_)])
            S.dma("sp", kT[s_], qk_own[128:256, tau0:tau0 + 64], reads=["qk_own"], writes=[("kT", s_)])
            S.dma("sp", tk[s_][0:64, :], tok_own[tau0:tau0 + 64, :], reads=["tok_own"], writes=[("tk", s_)])
            S.op("pe", lambda e, s_=s_: e.matmul(ps[2][0:64, 0:128], lhsT=glw[s_][0:16, :], rhs=wg2[0:16, :], start=True, stop=False),
                 reads=[("glw", s_), "wg2"], writes=[("ps", 2)])
            S.op("pe", lambda e: e.matmul(ps[2][0:64, 0:128], lhsT=self.ones_f[0:1, 0:64], rhs=bg[0:1, :], start=False, stop=True),
                 reads=["ones_f", "bgr"], writes=[("ps", 2)])
            S.op("act", lambda e: e.activation(out=sp_[0:64, :], in_=ps[2][0:64, 0:128], func=AF.Exp, scale=-1.0),
                 reads=[("ps", 2)], writes=["sp"])
            S.op("act", lambda e: e.activation(out=sp_[0:64, :], in_=sp_[0:64, :], func=AF.Ln, bias=1.0), reads=["sp"], writes=["sp"])
            S.op("pe", lambda e: e.matmul(ps[3][:, 0:64], lhsT=sp_[0:64, :], rhs=U[0:64, :], start=True, stop=True),
                 reads=["sp", "U"], writes=[("ps", 3)])
            S.op("pe", lambda e: e.matmul(ps[4][0:64, 0:128], lhsT=Lm[0:64, :], rhs=sp_[0:64, :], start=True, stop=True),
                 reads=["sp", "Lm"], writes=[("ps", 4)])
            S.op("act", lambda e: e.activation(out=E1, in_=ps[3][:, 0:64], func=AF.Exp), reads=[("ps", 3)], writes=["E1"])
            S.op("act", lambda e: e.activation(out=E2, in_=ps[3][:, 0:64], func=AF.Exp, scale=-1.0), reads=[("ps", 3)], writes=["E2"])
            S.op("act", lambda e: e.activation(out=E3[0:64, :], in_=ps[4][0:64, 0:128], func=AF.Exp), reads=[("ps", 4)], writes=["E3"])
            S.op("dve", lambda e, s_=s_: e.scalar_tensor_tensor(out=qd, in0=qT[s_], scalar=DKS, in1=E1, op0=ALU.mult, op1=ALU.mult),
                 reads=[("qT", s_), "E1"], writes=["qd"])
            S.op("dve", lambda e, s_=s_: e.tensor_tensor(out=kinv, in0=kT[s_], in1=E2, op=ALU.mult),
                 reads=[("kT", s_), "E2"], writes=["kinv"])
            S.op("dve", lambda e, s_=s_: e.tensor_tensor(out=kend[0:64, :], in0=tk[s_][0:64, 0:128], in1=E3[0:64, :], op=ALU.mult),
                 reads=[("tk", s_), "E3"], writes=["kend"])
            S.op("pe", lambda e: e.matmul(ps[5][0:64, 0:64], lhsT=kinv, rhs=qd, start=True, stop=True),
                 reads=["kinv", "qd"], writes=[("ps", 5)])
            S.op("dve", lambda e: e.tensor_tensor(out=attT[0:64, :], in0=ps[5][0:64, 0:64], in1=tri[0:64, :], op=ALU.mult),
                 reads=[("ps", 5), "tri"], writes=["attT"])
            S.op("pe", lambda e, s_=s_: e.matmul(ps[0][0:64, 0:256], lhsT=attT[0:64, :], rhs=tk[s_][0:64, 128:384], start=True, stop=False),
                 reads=["attT", ("tk", s_)], writes=[("ps", 0)])
            S.op("pe", lambda e: e.matmul(ps[0][0:64, 0:256], lhsT=qd, rhs=state_b, start=False, stop=True),
                 reads=["qd", "state_b"], writes=[("ps", 0)])
            S.op("pe", lambda e, s_=s_: e.matmul(ps[1][:, 0:256], lhsT=kend[0:64, :], rhs=tk[s_][0:64, 128:384], start=True, stop=True),
                 reads=["kend", ("tk", s_)], writes=[("ps", 1)])
            S.op("dve", lambda e: e.scalar_tensor_tensor(out=state, in0=state, scalar=E1[:, 63:64], in1=ps[1][:, 0:256],
                                                         op0=ALU.mult, op1=ALU.add),
                 reads=["state", "E1", ("ps", 1)], writes=["state"])
            S.op("dve", lambda e: e.tensor_copy(out=state_b, in_=state), reads=["state"], writes=["state_b"])
            S.op("act", lambda e: e.activation(out=junk[0:64, :], in_=ps[0][0:64, 0:256], func=AF.Square, accum_out=ssq[0:64, 0:1]),
                 reads=[("ps", 0)], writes=["junk", "ssq0"])
            S.op("act", lambda e: e.activation(out=ssq[0:64, 1:2], in_=ssq[0:64, 0:1], func=AF.Sqrt, scale=1.0 / 256, bias=EPS),
                 reads=["ssq0"], writes=["ssq1"])
            S.op("dve", lambda e: e.reciprocal(out=ssq[0:64, 2:3], in_=ssq[0:64, 1:2]), reads=["ssq1"], writes=["ssq2"])
            S.op("dve", lambda e: e.scalar_tensor_tensor(out=on[0:64, :], in0=ps[0][0:64, 0:256], scalar=ssq[0:64, 2:3],
                                                         in1=gout[0:64, :], op0=ALU.mult, op1=ALU.mult),
                 reads=[("ps", 0), "ssq2", "gout"], writes=["on"])
            for c2 in range(2):
                S.op("pe", lambda e, c2=c2: e.transpose(out=ps7b[:, c2 * 64:(c2 + 1) * 64], in_=on[0:64, c2 * 128:(c2 + 1) * 128],
                                                        identity=self.ident_b[0:64, 0:64]),
                     reads=["on", "ident_b"], writes=[("ps", 7)])
            oi = orot.next()
            S.op("act", lambda e, oi=oi: e.activation(out=ostg[oi], in_=ps7b[:, 0:128], func=AF.Copy),
                 reads=[("ps", 7)], writes=[("ostg", oi)])
            for c2 in range(2):
                S.dma("pool", gb[512 + c2 * 128:512 + (c2 + 1) * 128, tau0:tau0 + 64], ostg[oi][:, c2 * 64:(c2 + 1) * 64],
                      reads=[("ostg", oi)], writes=["gb"])
        if ("gbg", l) in self.taps:
            o = self.outp("tap_gbg_%d" % l, [256, SEQ], BF16)
            for r0 in range(0, 256, 128):
                S.dma("sp", o[r0:r0 + 128, :], gb[512 + r0:512 + r0 + 128, :], reads=["gb"])
        S.barrier()
        ar.release()

    def phase_G2(self, l):
        S = self.S
        self.GBg = self.gather_rows("gb%d" % l, self.gb, "gb", 768, SEQ, BF16, 64)
        yown = self.dram("yown_%d" % l, [4 * 768, NT], BF16)
        self.yown = yown

        def col0(e):
            if not hasattr(self, "_c0"):
                self._c0 = {}
            if id(e) not in self._c0:
                self._c0[id(e)] = self.rank_of(e) * 2048
            return self._c0[id(e)]

        for (g, r0, cr) in self.GBg:
            for rr in range(4):
                S.dma("pool", yown[rr * 768 + r0:rr * 768 + r0 + cr, :],
                      lambda e, g=g, rr=rr, cr=cr: g[rr * cr:(rr + 1) * cr, bass.ds(col0(e), 2048)],
                      reads=[("gb%d" % l, "g")], writes=["yown"])

    def phase_C(self, l, last):
        S, ar, ps = self.S, self.ar, self.ps
        ar.mark()
        A = self.A_out
        gbkey = ("gb%d" % l, "g")
        xsrc = self.x_own if l == 0 else self.dr["xres%d" % (l - 1)]
        if last:
            xdst = self.out_own
        else:
            xdst = self.dram("xres%d" % l, [NT, D], F32)
        yT = ar.alloc(32 * 512, BF16)
        yT3 = yT.rearrange("p (k t) -> p k t", k=32)
        mT = ar.alloc(32 * 512, BF16)
        mT3 = mT.rearrange("p (k t) -> p k t", k=32)
        wsl = [ar.alloc(32 * 512, BF16) for _ in range(2)]
        gate_bc = ar.alloc(D)
        S.dma("sp", gate_bc, self.dr["gatebc_%d" % l][:, :], reads=["gatebc_dram"], writes=["gate_bc"])
        if last:
            fg = ar.alloc(D)
            S.dma("sp", fg, self.final_g_in[0:1, :].partition_broadcast(128), writes=["fg"])
            xn = ar.alloc(D)
            junk = yT[:, 0:D]
            ss = ar.alloc(4)
        zt = [ar.alloc(512, BF16) for _ in range(2)]
        gt = [ar.alloc(3 * 512, BF16) for _ in range(2)]
        t1 = ar.alloc(512)
        t2 = ar.alloc(512)
        xc = [ar.alloc(512) for _ in range(2)]
        xo = [ar.alloc(512) for _ in range(2)]
        wrot, zrot, grot, xrot, brot = Rot(2), Rot(2), Rot(2), Rot(2), Rot(2)

        def my_rank(e):
            return self.rank_of(e)

        for tc in range(4):
            t0 = tc * 512
            for kc in range(32):
                if kc < 16:
                    rr, hh = kc // 4, kc % 4
                    S.dma("sp", yT3[:, kc, :], self.yown[rr * 768 + hh * 128:rr * 768 + (hh + 1) * 128, t0:t0 + 512],
                          reads=["yown"], writes=[("yT", kc)])
                elif kc < 24:
                    rr, c2 = (kc - 16) // 2, (kc - 16) % 2
                    S.dma("sp", yT3[:, kc, :], self.yown[rr * 768 + 512 + c2 * 128:rr * 768 + 512 + (c2 + 1) * 128, t0:t0 + 512],
                          reads=["yown"], writes=[("yT", kc)])
                else:
                    S.dma("sp", yT3[:, kc, :], self.odT[(kc - 24) * 128:(kc - 23) * 128, t0:t0 + 512], reads=["odT"],
                          writes=[("yT", kc)])
                zs = zrot.next()
                S.dma("sp", zt[zs], A["zT"][kc * 128:(kc + 1) * 128, t0:t0 + 512], reads=["zT"], writes=[("zt", zs)])
                S.op("pool", lambda e, kc=kc, zs=zs: e.tensor_tensor(out=yT3[:, kc, :], in0=yT3[:, kc, :], in1=zt[zs], op=ALU.mult),
                     reads=[("yT", kc), ("zt", zs)], writes=[("yT", kc)])
            yk = [("yT", kc) for kc in range(32)]
            for u in range(8):
                s_ = wrot.next()
                wbm = wsl[s_][:, 0:16 * 512].rearrange("p (k c) -> p k c", k=16)
                wbg = wsl[s_][:, 16 * 512:24 * 512].rearrange("p (k c) -> p k c", k=8)
                wbd = wsl[s_][:, 24 * 512:32 * 512].rearrange("p (k c) -> p k c", k=8)
                wkey = ("wsl", s_)
                self.load_unit("sp", wbm, "w_bm", l, u, 512, writes=[wkey])
                self.load_unit("sp", wbg, "w_bg", l, u, 512, writes=[wkey])
                self.load_unit("sp", wbd, "w_bd", l, u, 512, writes=[wkey])
                for pc in range(4):
                    dc = u * 4 + pc
                    gs = grot.next()
                    g3 = gt[gs].rearrange("p (b t) -> p b t", b=3)
                    for b_ in range(3):
                        S.dma("sp", g3[:, b_, :], A["gT"][b_ * D + dc * 128:b_ * D + (dc + 1) * 128, t0:t0 + 512], reads=["gT"],
                              writes=[("gt", gs)])
                    for (bank, w3, k0, nk) in ((0, wbm, 0, 16), (1, wbg, 16, 8), (2, wbd, 24, 8)):
                        for kk in range(nk):
                            S.op("pe", lambda e, bank=bank, w3=w3, kk=kk, k0=k0, nk=nk, pc=pc: e.matmul(
                                ps[bank][:, :], lhsT=w3[:, kk, pc * 128:(pc + 1) * 128], rhs=yT3[:, k0 + kk, :],
                                start=(kk == 0), stop=(kk == nk - 1)), reads=[wkey] + yk, writes=[("ps", bank)])
                    S.op("dve", lambda e, g3=g3: e.tensor_tensor(out=t1, in0=ps[0][:, :], in1=g3[:, 0, :], op=ALU.mult),
                         reads=[("ps", 0), ("gt", gs)], writes=["t1"])
                    S.op("dve", lambda e, g3=g3: e.tensor_tensor(out=t2, in0=ps[1][:, :], in1=g3[:, 1, :], op=ALU.mult),
                         reads=[("ps", 1), ("gt", gs)], writes=["t2"])
                    S.op("dve", lambda e: e.tensor_tensor(out=t1, in0=t1, in1=t2, op=ALU.add), reads=["t1", "t2"], writes=["t1"])
                    S.op("dve", lambda e, g3=g3: e.tensor_tensor(out=t2, in0=ps[2][:, :], in1=g3[:, 2, :], op=ALU.mult),
                         reads=[("ps", 2), ("gt", gs)], writes=["t2"])
                    S.op("dve", lambda e, dc=dc: e.tensor_tensor(out=mT3[:, dc, :], in0=t1, in1=t2, op=ALU.add),
                         reads=["t1", "t2"], writes=[("mT", dc)])
            mk = [("mT", dc) for dc in range(32)]
            for oc in range(8):
                s_ = wrot.next()
                wo = wsl[s_].rearrange("p (k c) -> p k c", k=32)
                wkey = ("wsl", s_)
                self.load_unit("sp", wo, "w_o", l, oc, 512, writes=[wkey])
                for tt in range(4):
                    u0 = t0 + tt * 128
                    bank = 3 + brot.next()
                    for dc in range(32):
                        S.op("pe", lambda e, dc=dc, tt=tt, bank=bank, wo=wo: e.matmul(
                            ps[bank][:, :], lhsT=mT3[:, dc, tt * 128:(tt + 1) * 128], rhs=wo[:, dc, :],
                            start=(dc == 0), stop=(dc == 31)), reads=[wkey] + mk, writes=[("ps", bank)])
                    xs = xrot.next()
                    S.dma("sp", xc[xs], xsrc[u0:u0 + 128, oc * 512:(oc + 1) * 512], writes=[("xc", xs)])
                    S.op("dve", lambda e, bank=bank, oc=oc: e.tensor_tensor(out=t1, in0=ps[bank][:, :],
                                                                          in1=gate_bc[:, oc * 512:(oc + 1) * 512], op=ALU.mult),
                         reads=[("ps", bank), "gate_bc"], writes=["t1"])
                    S.op("pool", lambda e, xs=xs: e.tensor_tensor(out=xo[xs], in0=t1, in1=xc[xs], op=ALU.add),
                         reads=["t1", ("xc", xs)], writes=[("xo", xs)])
                    S.dma("pool", self.xmid[u0:u0 + 128, oc * 512:(oc + 1) * 512] if last else xdst[u0:u0 + 128, oc * 512:(oc + 1) * 512],
                          xo[xs], reads=[("xo", xs)], writes=["xout"])
            if last:
                for tt in range(4):
                    u0 = t0 + tt * 128
                    for hh in range(2):
                        S.dma("sp", xn[:, hh * 2048:(hh + 1) * 2048], self.xmid[u0:u0 + 128, hh * 2048:(hh + 1) * 2048],
                              reads=["xout"], writes=["xn"])
                    S.op("act", lambda e: e.activation(out=junk, in_=xn, func=AF.Square, accum_out=ss[:, 0:1]),
                         reads=["xn"], writes=[("yT", kc_) for kc_ in range(8)] + ["ss0"])
                    S.op("act", lambda e: e.activation(out=ss[:, 1:2], in_=ss[:, 0:1], func=AF.Sqrt, scale=1.0 / D, bias=EPS),
                         reads=["ss0"], writes=["ss1"])
                    S.op("dve", lambda e: e.reciprocal(out=ss[:, 2:3], in_=ss[:, 1:2]), reads=["ss1"], writes=["ss2"])
                    S.op("dve", lambda e: e.scalar_tensor_tensor(out=xn, in0=xn, scalar=ss[:, 2:3], in1=fg, op0=ALU.mult, op1=ALU.mult),
                         reads=["xn", "ss2", "fg"], writes=["xn"])
                    for hh in range(2):
                        S.dma("pool", self.out_own[u0:u0 + 128, hh * 2048:(hh + 1) * 2048], xn[:, hh * 2048:(hh + 1) * 2048],
                              reads=["xn"], writes=["final"])
        if ("x1", l) in self.taps:
            o = self.outp("tap_x1_%d" % l, [512, D])
            for r0 in range(0, 512, 128):
                S.dma("sp", o[r0:r0 + 128, :], xdst[r0:r0 + 128, :], reads=["xout"])
        S.barrier()
        ar.release()


def perm_tokens():
    r = np.arange(4)[:, None, None]
    i = np.arange(16)[None, :, None]
    p = np.arange(128)[None, None, :]
    return ((4 * i + r) * 128 + p).reshape(-1)


def col_layout(v):
    return np.ascontiguousarray(v.reshape(-1, 128).T)


def make_in_maps(inputs, layers=(0, 1)):
    perm = perm_tokens()
    inv = (10000.0 ** (-np.arange(0, 64, 2, dtype=np.float32) / 64)).astype(np.float32)
    maps = []
    for c in range(8):
        g, r = c // 4, c % 4
        sh = 2 * r + g
        m = {}
        own = perm[r * 2048:(r + 1) * 2048]
        m["x_own"] = np.ascontiguousarray(inputs["x"][g][own])
        m["c_own"] = col_layout(inputs["c"][g])
        m["pos_perm"] = np.ascontiguousarray(inputs["positions"][g][perm][None, :]).astype(np.int32)
        cols = np.zeros((128, NCOL), np.float32)
        cols[:, COL_INV] = inv[np.arange(128) % 32]
        cols[:, COL_SGN] = np.where((np.arange(128) % 64) < 32, -1.0, 1.0)
        cols[:, COL_NEGPI] = -np.pi
        for l in layers:
            b = COL_L0 + l * COL_PER_L
            cols[:, b + COL_NORMG:b + COL_NORMG + 32] = col_layout(inputs["norm_g"][l])
            cols[:, b + COL_BSHIFT:b + COL_BSHIFT + 32] = col_layout(inputs["b_ada"][l][0:D])
            cols[:, b + COL_BSCALE:b + COL_BSCALE + 32] = col_layout(inputs["b_ada"][l][D:2 * D])
            cols[:, b + COL_BMG:b + COL_BMG + 96] = col_layout(inputs["b_mg"][l])
            cols[:, b + COL_GQ:b + COL_GQ + 6] = col_layout(inputs["mla_gq"][l])
            cols[:, b + COL_GKV:b + COL_GKV + 4] = col_layout(inputs["mla_gkv"][l])
            m["bgate_%d" % l] = np.ascontiguousarray(inputs["b_ada"][l][None, 2 * D:3 * D])
            for name, (K, N, units) in TILED.items():
                R = K // 8
                m["%s_%d" % (name, l)] = np.ascontiguousarray(inputs[name][l][sh * R:(sh + 1) * R])
            hs = [4 * r + i for i in range(4)]
            wuq = inputs["mla_wuq"][l].reshape(768, 16, 192)[:, hs, :]
            wuq = np.concatenate([wuq, wuq[:, :, 160:192], wuq[:, :, 128:160]], axis=2)
            m["wuq_own_%d" % l] = np.ascontiguousarray(wuq.reshape(768, 1024))
            wukv = inputs["mla_wukv"][l].reshape(512, 16, 256)[:, hs, :]
            m["wukv_own_%d" % l] = np.ascontiguousarray(wukv.reshape(512, 1024))
        sidx = np.arange(64)[:, None]
        cidx = np.arange(64)[None, :]
        m["gla_consts"] = np.concatenate([np.where(sidx <= cidx, -1.0 / 16, 0.0), np.where(sidx > cidx, -1.0 / 16, 0.0),
                                          np.where(sidx <= cidx, 1.0, 0.0)], axis=1).astype(np.float32)
        m["final_g_row"] = np.ascontiguousarray(inputs["final_g"][None, :])
        for l in layers:
            m["gla_wg2_%d" % l] = np.ascontiguousarray(np.concatenate(
                [inputs["gla_wg2"][l][:, r * 128:(r + 1) * 128], inputs["gla_bg"][l][None, r * 128:(r + 1) * 128]], axis=0))
            m["gla_gout_%d" % l] = np.ascontiguousarray(inputs["gla_gout"][l][None, :])
        m["cols"] = cols
        m["ident"] = np.eye(128, dtype=np.float32)
        import ml_dtypes
        kp = np.arange(128)[:, None, None, None]
        rkk = np.arange(4)[None, :, None, None]
        rq = np.arange(4)[None, None, :, None]
        qp = np.arange(128)[None, None, None, :]
        mm = ((rkk < rq) | ((rkk == rq) & (kp <= qp))).astype(np.float32)
        m["mla_mask"] = mm.reshape(128, 2048).astype(ml_dtypes.bfloat16)
        qq = np.arange(128)[:, None, None]
        rk3 = np.arange(4)[None, :, None]
        kk = np.arange(128)[None, None, :]
        vis = ((rk3 < r) | ((rk3 == r) & (kk <= qq))).reshape(128, 512)
        m["dsa_dmask"] = np.where(vis, 0.0, -1e30).astype(np.float32)
        m["dsa_c01"] = vis.astype(np.float32).astype(ml_dtypes.bfloat16)
        maps.append(m)
    return maps


_CACHE = {}


def kernel(**inputs):
    inputs = {k: np.asarray(v) for k, v in inputs.items()}
    if "nc" not in _CACHE:
        B = Builder(layers=(0, 1))
        _CACHE["nc"] = B.build()
        _CACHE["B"] = B
    B, nc = _CACHE["B"], _CACHE["nc"]
    maps = make_in_maps(inputs, layers=(0, 1))
    maps = [{k: v for k, v in m.items() if k in B.ins} for m in maps]
    res = run_bass_kernel_spmd(nc, maps, core_ids=list(range(8)))
    perm = perm_tokens()
    out = np.empty((2, SEQ, D), np.float32)
    for c in range(8):
        g, r = c // 4, c % 4
        out[g][perm[r * 2048:(r + 1) * 2048]] = np.asarray(res.results[c]["out_own"], dtype=np.float32)
    return out
```
